# Optimizing a Trainium2 kernel written in Bass

```python
import math
import jax, jax.numpy as jnp
from jax import lax
import numpy as np

D_MODEL = 1024
BATCH = 16
SEQ = 2048
DEPTH = 1

PLE_DIM = 256
EPS = 1e-6

GLA_HEADS = 4
GLA_DK = 64
GLA_DV = 128
GLA_LOWRANK = 16
GLA_TAU = 16.0
GLA_CHUNK = 64

DIFF_HEADS = 4
DIFF_DH = 64
DIFF_DV = 2 * DIFF_DH
ROPE_THETA = 500000.0
ROPE_DIM = DIFF_DH // 4
Q_BLOCK = 128

N_GROUPS = 4
EXPERTS_PER_GROUP = 8
N_EXPERTS = N_GROUPS * EXPERTS_PER_GROUP
TOP_K = 2
D_EXPERT = 256

SPLIT_SIZES = (
    GLA_HEADS * GLA_DK,
    GLA_HEADS * GLA_DK,
    GLA_HEADS * GLA_DV,
    GLA_HEADS * GLA_DV,
    GLA_LOWRANK,
    DIFF_HEADS * 2 * DIFF_DH,
    DIFF_HEADS * 2 * DIFF_DH,
    DIFF_HEADS * DIFF_DV,
    D_MODEL,
    D_MODEL,
)
D_IN = sum(SPLIT_SIZES)

kernel_name = "hybrid_gla_diffattn_hmoe_block"


def rmsnorm(x, g):
    xf = x.astype(jnp.float32)
    y = xf * lax.rsqrt(jnp.mean(xf * xf, axis=-1, keepdims=True) + EPS)
    return (y * g.astype(jnp.float32)).astype(x.dtype)


def rope_partial(x, pos):
    half = ROPE_DIM // 2
    inv = ROPE_THETA ** (-jnp.arange(0, ROPE_DIM, 2, dtype=jnp.float32) / ROPE_DIM)
    ang = pos.astype(jnp.float32)[..., None] * inv
    cos = jnp.cos(ang)[:, :, None, :]
    sin = jnp.sin(ang)[:, :, None, :]
    xr = x[..., :ROPE_DIM].astype(jnp.float32)
    x1, x2 = xr[..., :half], xr[..., half:]
    rot = jnp.concatenate([x1 * cos - x2 * sin, x2 * cos + x1 * sin], axis=-1)
    return jnp.concatenate([rot.astype(x.dtype), x[..., ROPE_DIM:]], axis=-1)


def gla_branch(q, k, v, log_a):
    B, S, H, DK = q.shape
    DV = v.shape[-1]
    N = S // GLA_CHUNK

    def chunks(t):
        return t.astype(jnp.float32).reshape(B, N, GLA_CHUNK, H, t.shape[-1]).transpose(0, 3, 1, 2, 4)

    qc, kc, vc, lac = chunks(q), chunks(k), chunks(v), chunks(log_a)
    b = jnp.cumsum(lac, axis=3)
    b_last = b[:, :, :, -1, :]
    q_dec = qc * jnp.exp(b)
    k_inv = kc * jnp.exp(-b)
    causal = jnp.tril(jnp.ones((GLA_CHUNK, GLA_CHUNK), dtype=bool))
    att = jnp.einsum('bhncd,bhnsd->bhncs', q_dec, k_inv)
    att = jnp.where(causal, att, 0.0)
    o_intra = jnp.einsum('bhncs,bhnsv->bhncv', att, vc)

    k_end = kc * jnp.exp(b_last[:, :, :, None, :] - b)
    u = jnp.einsum('bhncd,bhncv->bhndv', k_end, vc)
    decay = jnp.exp(b_last)

    def step(state, inp):
        dec, un = inp
        return state * dec[..., None] + un, state

    s0 = jnp.zeros((B, H, DK, DV), jnp.float32)
    _, s_prev = lax.scan(step, s0, (jnp.moveaxis(decay, 2, 0), jnp.moveaxis(u, 2, 0)))
    s_prev = jnp.moveaxis(s_prev, 0, 2)
    o_inter = jnp.einsum('bhncd,bhndv->bhncv', q_dec, s_prev)
    o = o_intra + o_inter
    return o.transpose(0, 2, 3, 1, 4).reshape(B, S, H, DV)


def diff_attention(q, k, v, lam):
    B, H, _, S, Dh = q.shape
    DV = v.shape[-1]
    nb = S // Q_BLOCK
    scale = Dh ** -0.5
    qb = q.reshape(B, H, 2, nb, Q_BLOCK, Dh).transpose(3, 0, 1, 2, 4, 5)
    kpos = jnp.arange(S)
    neg = jnp.finfo(jnp.float32).min

    def block(args):
        qblk, i = args
        s = jnp.einsum('bhcqd,bhckd->bhcqk', qblk, k).astype(jnp.float32) * scale
        qpos = i * Q_BLOCK + jnp.arange(Q_BLOCK)
        mask = qpos[:, None] >= kpos[None, :]
        s = jnp.where(mask, s, neg)
        a = jax.nn.softmax(s, axis=-1)
        w = a[:, :, 0] - lam * a[:, :, 1]
        return jnp.einsum('bhqk,bhkv->bhqv', w.astype(v.dtype), v)

    out = lax.map(block, (qb, jnp.arange(nb)))
    return out.transpose(1, 0, 3, 2, 4).reshape(B, S, H, DV)


def token_mixers(n, positions, w_in, w_a2, b_a, gla_norm, lq1, lk1, lq2, lk2,
                 diff_subln, w_branch_a, w_branch_b, w_out, lambda_init):
    B, S, _ = n.shape
    proj = n @ w_in
    idx = [int(c) for c in np.cumsum(SPLIT_SIZES)[:-1]]
    (g_q, g_k, g_v, g_r, g_al, d_q, d_k, d_v, gate_a, gate_b) = jnp.split(proj, idx, axis=-1)

    gq = g_q.reshape(B, S, GLA_HEADS, GLA_DK) * (GLA_DK ** -0.5)
    gk = g_k.reshape(B, S, GLA_HEADS, GLA_DK)
    gv = g_v.reshape(B, S, GLA_HEADS, GLA_DV)
    a_logit = (g_al @ w_a2 + b_a).astype(jnp.float32)
    log_a = (jax.nn.log_sigmoid(a_logit) / GLA_TAU).reshape(B, S, GLA_HEADS, GLA_DK)
    o_a = gla_branch(gq, gk, gv, log_a)
    o_a = rmsnorm(o_a, gla_norm).reshape(B, S, GLA_HEADS * GLA_DV).astype(n.dtype)
    o_a = o_a * jax.nn.silu(g_r)
    y_a = o_a @ w_branch_a

    dq = rope_partial(d_q.reshape(B, S, DIFF_HEADS * 2, DIFF_DH), positions)
    dk = rope_partial(d_k.reshape(B, S, DIFF_HEADS * 2, DIFF_DH), positions)
    dq = dq.reshape(B, S, DIFF_HEADS, 2, DIFF_DH).transpose(0, 2, 3, 1, 4)
    dk = dk.reshape(B, S, DIFF_HEADS, 2, DIFF_DH).transpose(0, 2, 3, 1, 4)
    dv = d_v.reshape(B, S, DIFF_HEADS, DIFF_DV).transpose(0, 2, 1, 3)
    lam = (jnp.exp(jnp.sum(lq1.astype(jnp.float32) * lk1.astype(jnp.float32)))
           - jnp.exp(jnp.sum(lq2.astype(jnp.float32) * lk2.astype(jnp.float32)))
           + lambda_init)
    o_b = diff_attention(dq, dk, dv, lam)
    o_b = rmsnorm(o_b, diff_subln) * (1.0 - lambda_init)
    y_b = o_b.reshape(B, S, DIFF_HEADS * DIFF_DV).astype(n.dtype) @ w_branch_b

    merged = jax.nn.sigmoid(gate_a) * y_a + jax.nn.sigmoid(gate_b) * y_b
    return merged @ w_out


def hierarchical_moe(n, w_rg, b_rg, w_re, b_re, w_gate, w_up, w_down):
    B, S, D = n.shape
    xt = n.reshape(B * S, D)
    T = xt.shape[0]
    g_logits = (xt @ w_rg + b_rg).astype(jnp.float32)
    g_prob = jax.nn.softmax(g_logits, axis=-1)
    g_p, g_idx = lax.top_k(g_prob, 1)
    e_logits = (xt @ w_re + b_re).astype(jnp.float32).reshape(T, N_GROUPS, EXPERTS_PER_GROUP)
    e_logits = jnp.take_along_axis(e_logits, g_idx[:, :, None], axis=1)[:, 0]
    e_prob = jax.nn.softmax(e_logits, axis=-1)
    e_p, e_idx = lax.top_k(e_prob, TOP_K)
    e_p = e_p / jnp.sum(e_p, axis=-1, keepdims=True)
    weights = g_p * e_p
    eid = g_idx * EXPERTS_PER_GROUP + e_idx
    comb = jnp.sum(jax.nn.one_hot(eid, N_EXPERTS, dtype=jnp.float32) * weights[..., None], axis=1)
    comb = comb.astype(n.dtype)
    y = jnp.zeros_like(xt)
    for e in range(N_EXPERTS):
        he = jax.nn.silu(xt @ w_gate[e]) * (xt @ w_up[e])
        y = y + comb[:, e:e + 1] * (he @ w_down[e])
    return y.reshape(B, S, D)


def setup_inputs(seed: int = 0) -> dict:
    key = jax.random.key(seed)
    ks = iter(jax.random.split(key, 40))

    def nrm(shape, fan_in):
        return jax.random.normal(next(ks), shape, jnp.float32) * (fan_in ** -0.5)

    def gain(shape):
        return 1.0 + 0.01 * jax.random.normal(next(ks), shape, jnp.float32)

    x = jax.random.normal(next(ks), (BATCH, SEQ, D_MODEL), jnp.float32)
    p = jax.random.normal(next(ks), (DEPTH, BATCH, SEQ, PLE_DIM), jnp.float32)
    offs = jax.random.randint(next(ks), (BATCH, 1), 0, 4096, dtype=jnp.int32)
    positions = offs + jnp.arange(SEQ, dtype=jnp.int32)[None, :]
    return {
        "x": x,
        "p": p,
        "positions": positions,
        "attn_norm": gain((DEPTH, D_MODEL)),
        "w_in": nrm((DEPTH, D_MODEL, D_IN), D_MODEL),
        "w_a2": nrm((DEPTH, GLA_LOWRANK, GLA_HEADS * GLA_DK), GLA_LOWRANK),
        "b_a": 0.1 * jax.random.normal(next(ks), (DEPTH, GLA_HEADS * GLA_DK), jnp.float32),
        "gla_norm": gain((DEPTH, GLA_DV)),
        "lambda_q1": 0.1 * jax.random.normal(next(ks), (DEPTH, DIFF_DH), jnp.float32),
        "lambda_k1": 0.1 * jax.random.normal(next(ks), (DEPTH, DIFF_DH), jnp.float32),
        "lambda_q2": 0.1 * jax.random.normal(next(ks), (DEPTH, DIFF_DH), jnp.float32),
        "lambda_k2": 0.1 * jax.random.normal(next(ks), (DEPTH, DIFF_DH), jnp.float32),
        "diff_subln": gain((DEPTH, DIFF_DV)),
        "w_branch_a": nrm((DEPTH, GLA_HEADS * GLA_DV, D_MODEL), GLA_HEADS * GLA_DV),
        "w_branch_b": nrm((DEPTH, DIFF_HEADS * DIFF_DV, D_MODEL), DIFF_HEADS * DIFF_DV),
        "w_out": nrm((DEPTH, D_MODEL, D_MODEL), D_MODEL),
        "ffn_norm": gain((DEPTH, D_MODEL)),
        "w_router_group": nrm((DEPTH, D_MODEL, N_GROUPS), D_MODEL),
        "b_router_group": 0.01 * jax.random.normal(next(ks), (DEPTH, N_GROUPS), jnp.float32),
        "w_router_expert": nrm((DEPTH, D_MODEL, N_EXPERTS), D_MODEL),
        "b_router_expert": 0.01 * jax.random.normal(next(ks), (DEPTH, N_EXPERTS), jnp.float32),
        "w_gate": nrm((DEPTH, N_EXPERTS, D_MODEL, D_EXPERT), D_MODEL),
        "w_up": nrm((DEPTH, N_EXPERTS, D_MODEL, D_EXPERT), D_MODEL),
        "w_down": nrm((DEPTH, N_EXPERTS, D_EXPERT, D_MODEL), D_EXPERT),
        "w_ple": nrm((DEPTH, PLE_DIM, D_MODEL), PLE_DIM),
        "ple_norm": gain((DEPTH, D_MODEL)),
        "w_ple_gate": nrm((DEPTH, D_MODEL, D_MODEL), D_MODEL),
        "final_norm": gain((D_MODEL,)),
    }


def reference(x, p, positions, attn_norm, w_in, w_a2, b_a, gla_norm,
              lambda_q1, lambda_k1, lambda_q2, lambda_k2, diff_subln,
              w_branch_a, w_branch_b, w_out, ffn_norm,
              w_router_group, b_router_group, w_router_expert, b_router_expert,
              w_gate, w_up, w_down, w_ple, ple_norm, w_ple_gate, final_norm):
    h = x
    for i in range(DEPTH):
        lambda_init = 0.8 - 0.6 * math.exp(-0.3 * i)
        n = rmsnorm(h, attn_norm[i])
        h = h + token_mixers(n, positions, w_in[i], w_a2[i], b_a[i], gla_norm[i],
                             lambda_q1[i], lambda_k1[i], lambda_q2[i], lambda_k2[i],
                             diff_subln[i], w_branch_a[i], w_branch_b[i], w_out[i],
                             lambda_init)
        n2 = rmsnorm(h, ffn_norm[i])
        h = h + hierarchical_moe(n2, w_router_group[i], b_router_group[i],
                                 w_router_expert[i], b_router_expert[i],
                                 w_gate[i], w_up[i], w_down[i])
        e = rmsnorm(p[i] @ w_ple[i], ple_norm[i])
        h = h + jax.nn.sigmoid(h @ w_ple_gate[i]) * e
    return rmsnorm(h, final_norm)
```

```python
import math
import os
from contextlib import ExitStack

import numpy as np
import concourse.bass as bass
import concourse.mybir as mybir
from concourse.bass_utils import run_bass_kernel_spmd

F32 = mybir.dt.float32
BF16 = mybir.dt.bfloat16
I32 = mybir.dt.int32
ALU = mybir.AluOpType
AF = mybir.ActivationFunctionType
AX = mybir.AxisListType

D = 1024
DIN = 5136
NE = 32
EPS = 1e-6
LAMBDA_INIT = 0.8 - 0.6 * math.exp(-0.3 * 0)
MAGIC = 12582912.0
PI_LO = 3.1415925
NCST = 656
SK = os.environ.get("KSKIP", "")

COMPUTE = ("pe", "act", "dve", "pool")
SAME_ENGINE_SYNC = {"pe": False, "act": True, "dve": True, "pool": True, "sp": False}


class Op:
    __slots__ = ("eng", "fn", "reads", "writes", "dma", "waits", "dma_waits",
                 "signal", "token", "dma_val")

    def __init__(self, eng, fn, reads, writes, dma):
        self.eng = eng
        self.fn = fn
        self.reads = reads
        self.writes = writes
        self.dma = dma
        self.waits = []
        self.dma_waits = []
        self.signal = False
        self.token = 0
        self.dma_val = 0


class Prog:
    def __init__(self, nc):
        self.nc = nc
        self.ops = []
        self.last_writer = {}
        self.readers = {}
        self.dma_count = {}
        self.known = {e: {} for e in ("pe", "act", "dve", "pool", "sp")}
        self.snap = []

    def add(self, eng, fn, reads=(), writes=(), dma=None):
        i = len(self.ops)
        op = Op(eng, fn, tuple(reads), tuple(writes), dma)
        deps = set()
        for k in op.reads:
            w = self.last_writer.get(k)
            if w is not None:
                deps.add(w)
        for k in op.writes:
            w = self.last_writer.get(k)
            if w is not None:
                deps.add(w)
            r = self.readers.get(k)
            if r:
                deps.update(r)
        known = self.known[eng]
        need = {}
        for j in deps:
            pj = self.ops[j]
            if pj.dma is not None:
                key = ("dma", pj.dma)
                if known.get(key, 0) < pj.dma_val and need.get(key, 0) < pj.dma_val:
                    need[key] = pj.dma_val
            else:
                if pj.eng == eng and not SAME_ENGINE_SYNC[eng]:
                    continue
                if known.get(pj.eng, -1) < j and need.get(pj.eng, -1) < j:
                    need[pj.eng] = j
        for key, v in need.items():
            if isinstance(key, tuple):
                op.dma_waits.append((key[1], v))
                known[key] = v
            else:
                op.waits.append(v)
                self.ops[v].signal = True
                known[key] = v
                for kk, vv in self.snap[v].items():
                    if known.get(kk, -1) < vv:
                        known[kk] = vv
        if dma is not None:
            self.dma_count[dma] = self.dma_count.get(dma, 0) + 16
            op.dma_val = self.dma_count[dma]
        self.snap.append(dict(known))
        for k in op.reads:
            self.readers.setdefault(k, []).append(i)
        for k in op.writes:
            self.last_writer[k] = i
            self.readers[k] = []
        self.ops.append(op)
        return i

    def pe(self, fn, reads=(), writes=()):
        return self.add("pe", fn, reads, writes)

    def act(self, fn, reads=(), writes=()):
        return self.add("act", fn, reads, writes)

    def dve(self, fn, reads=(), writes=()):
        return self.add("dve", fn, reads, writes)

    def pool(self, fn, reads=(), writes=()):
        return self.add("pool", fn, reads, writes)

    def dma(self, queue, fn, key, reads=(), writes=()):
        return self.add(queue, fn, reads, writes, dma=key)

    def barrier(self, scratch):
        keys = set(self.last_writer) | set(self.readers)
        keys.add("__bar")
        self.add("pool", lambda e: e.memset(scratch, 0.0), reads=(), writes=tuple(keys))
        for eng in ("pe", "act", "dve", "sp"):
            self.add(eng, None, reads=("__bar",))
        self.last_writer = {"__bar": self.last_writer["__bar"]}
        self.readers = {}

    def emit(self, final_keys):
        nc = self.nc
        self.add("sp", None, reads=tuple(final_keys), writes=())
        counts = {e: 0 for e in self.known}
        for op in self.ops:
            if op.signal:
                counts[op.eng] += 1
                op.token = counts[op.eng]
        with ExitStack() as es:
            sems = {e: es.enter_context(nc.semaphore("s_" + e)) for e in COMPUTE}
            dsems = {}
            for k in self.dma_count:
                dsems[k] = es.enter_context(nc.semaphore("d_%d" % len(dsems)))
            block = es.enter_context(nc.Block())
            ops = self.ops

            def run(eng_name):
                def body(e):
                    for op in ops:
                        if op.eng != eng_name:
                            continue
                        for j in op.waits:
                            pj = ops[j]
                            e.wait_ge(sems[pj.eng], pj.token)
                        for (k, v) in op.dma_waits:
                            e.wait_ge(dsems[k], v)
                        if op.fn is None:
                            continue
                        ins = op.fn(e)
                        if op.dma is not None:
                            ins.then_inc(dsems[op.dma], 16)
                        elif op.signal:
                            ins.then_inc(sems[op.eng], 1)
                return body

            block.tensor(run("pe"))
            block.scalar(run("act"))
            block.vector(run("dve"))
            block.gpsimd(run("pool"))
            block.sync(run("sp"))
        return counts


def make_consts():
    c = np.zeros((128, NCST), np.float32)
    i = np.arange(128)
    c[:, 0:128] = np.eye(128, dtype=np.float32)
    c[:, 128:256] = (i[:, None] <= i[None, :]).astype(np.float32)
    same = (i[:, None] // 64) == (i[None, :] // 64)
    bd = (same & (i[:, None] <= i[None, :])).astype(np.float32)
    c[:, 256:384] = bd
    c[:, 384:512] = -bd / 16.0
    c[:, 512:640] = -(same & (i[:, None] > i[None, :])).astype(np.float32) / 16.0
    inv = 500000.0 ** (-np.arange(0, 16, 2, dtype=np.float32) / 16.0)
    c[:, 640:648] = inv.astype(np.float32)[None, :]
    for h in range(4):
        c[:, 648 + h] = ((i // 64) == (h % 2)).astype(np.float32)
    return c


def build(T=2048, NSEQ=2, dbg=None, NEX=NE, STOP=None):
    NT = T // 128
    NG = T // 512
    nc = bass.Bass("TRN2", target_bir_lowering=False)

    def din(name, shape, dt=F32):
        return nc.dram_tensor(name, list(shape), dt, kind="ExternalInput").ap()

    x_d = din("x", [NSEQ, T, D])
    p_d = din("p", [NSEQ, T, 256])
    pos_d = din("pos", [NSEQ, T], I32)
    attn_norm_d = din("attn_norm", [D])
    w_in_d = din("w_in", [D, DIN])
    w_a2_d = din("w_a2", [16, 256])
    b_a_d = din("b_a", [256])
    gla_norm_d = din("gla_norm", [128])
    lq1_d = din("lq1", [64]); lk1_d = din("lk1", [64]); lq2_d = din("lq2", [64]); lk2_d = din("lk2", [64])
    subln_d = din("diff_subln", [128])
    w_ba_d = din("w_ba", [512, D])
    w_bb_d = din("w_bb", [512, D])
    w_out_d = din("w_out", [D, D])
    ffn_norm_d = din("ffn_norm", [D])
    w_rg_d = din("w_rg", [D, 4]); b_rg_d = din("b_rg", [4])
    w_re_d = din("w_re", [D, 32]); b_re_d = din("b_re", [32])
    w_gate_d = din("w_gate", [NE, D, 256])
    w_up_d = din("w_up", [NE, D, 256])
    w_down_d = din("w_down", [NE, 256, D])
    w_ple_d = din("w_ple", [256, D])
    ple_norm_d = din("ple_norm", [D])
    w_pg_d = din("w_pg", [D, D])
    final_norm_d = din("final_norm", [D])
    cst_d = din("cst", [128, NCST])
    out_d = nc.dram_tensor("out", [NSEQ, T, D], F32, kind="ExternalOutput").ap()
    dbg_outs = {}

    P = Prog(nc)
    top = ExitStack()

    nsb = [0]

    def sb(name, shape, dt, es=None):
        nsb[0] += 1
        return (es or top).enter_context(nc.sbuf_tensor("sb%d_%s" % (nsb[0], name), list(shape), dt))

    def cw(ap):
        return ap.rearrange("(c p) n -> p c n", p=128)

    def row(ap):
        return ap.rearrange("(o n) -> o n", o=1)

    with top:
        PS2 = [top.enter_context(nc.psum_tensor("ps2_%d" % i, [128, 1024], F32)) for i in range(2)]
        PS1 = [top.enter_context(nc.psum_tensor("ps1_%d" % i, [128, 512], F32)) for i in range(2)]
        PT = [top.enter_context(nc.psum_tensor("pst_%d" % i, [128, 1024], BF16)) for i in range(2)]
        BANK = [PS2[0][:, 0:512], PS2[0][:, 512:1024], PS2[1][:, 0:512], PS2[1][:, 512:1024],
                PS1[0][:, :], PS1[1][:, :]]

        def BK(i):
            return ("B", i)

        def PTK(i):
            return ("PT", i)

        cst = sb("cst", [128, NCST], F32)
        ident_f = cst[:, 0:128]
        caus_f = cst[:, 128:256]
        bd_f = cst[:, 256:384]
        tri_f = cst[:, 384:512]
        trr_f = cst[:, 512:640]
        invf = cst[:, 640:648]
        rowmask4 = cst[:, 648:652]
        ident_bf = sb("ident_bf", [128, 128], BF16)
        caus_bf = sb("caus_bf", [128, 128], BF16)
        ones_bf = sb("ones_bf", [128, 128], BF16)
        maskneg_bf = sb("maskneg_bf", [128, 128], BF16)
        zeros_bf = sb("zeros_bf", [128, 512], BF16)
        scr = sb("scr", [128, 4], F32)
        wal_pad = sb("wal_pad", [128, 8, 128], BF16)
        wa2_pad = sb("wa2_pad", [128, 256], BF16)
        ba_pad = sb("ba_pad", [128, 256], BF16)
        wr_bf = sb("wr_bf", [128, 8, 36], BF16)
        rb_pad = sb("rb_pad", [128, 36], BF16)
        ggla = sb("ggla", [128, 128], F32)
        gsub = sb("gsub", [128, 128], F32)
        lqk = sb("lqk", [128, 4, 64], F32)
        lam_t = sb("lam_t", [128, 8], F32)
        gA = sb("gA", [128, D], F32)
        gB = sb("gB", [128, D], F32)
        nT = sb("nT", [128, 8, T], BF16)
        oaT = sb("oaT", [128, 4, T], BF16)
        obT = sb("obT", [128, 4, T], BF16)
        cosb = sb("cosb", [128, NT, 8], F32)
        sinb = sb("sinb", [128, NT, 8], F32)
        pos_i = sb("pos_i", [128, NT], I32)
        pos_f = sb("pos_f", [128, NT], F32)
        ang = sb("ang", [128, NT, 8], F32)
        ang2 = sb("ang2", [128, NT, 8], F32)
        comb = sb("comb", [128, NT, 32], F32)

        P.dma("sp", lambda e: e.dma_start(out=cst[:], in_=cst_d), "cst", writes=["cst"])
        P.dve(lambda e: e.tensor_copy(out=ident_bf[:], in_=ident_f), reads=["cst"], writes=["ident_bf"])
        P.dve(lambda e: e.tensor_copy(out=caus_bf[:], in_=caus_f), reads=["cst"], writes=["caus_bf"])
        P.pool(lambda e: e.memset(ones_bf[:], 1.0), writes=["ones_bf"])
        P.dve(lambda e: e.tensor_scalar(out=maskneg_bf[:], in0=caus_f, scalar1=-1.0, scalar2=30000.0, op0=ALU.add, op1=ALU.mult), reads=["cst"], writes=["maskneg"])
        P.pool(lambda e: e.memset(zeros_bf[:], 0.0), writes=["zeros_bf"])
        P.pool(lambda e: e.memset(wal_pad[:], 0.0), writes=["wal_pad"])
        P.pool(lambda e: e.memset(wa2_pad[:], 0.0), writes=["wa2_pad"])
        P.pool(lambda e: e.memset(ba_pad[:], 0.0), writes=["ba_pad"])
        P.pool(lambda e: e.memset(rb_pad[:], 0.0), writes=["rb_pad"])
        P.dma("pool", lambda e: e.dma_start(out=wal_pad[:, :, 0:16], in_=cw(w_in_d[:, 1536:1552])), "wal", writes=["wal_pad"])
        P.dma("pool", lambda e: e.dma_start(out=wa2_pad[0:16, :], in_=w_a2_d), "wa2", writes=["wa2_pad"])
        P.dma("pool", lambda e: e.dma_start(out=ba_pad[0:1, :], in_=row(b_a_d)), "ba", writes=["ba_pad"])
        P.dma("pool", lambda e: e.dma_start(out=wr_bf[:, :, 0:4], in_=cw(w_rg_d)), "wr", writes=["wr_bf"])
        P.dma("pool", lambda e: e.dma_start(out=wr_bf[:, :, 4:36], in_=cw(w_re_d)), "wr2", writes=["wr_bf"])
        P.dma("pool", lambda e: e.dma_start(out=rb_pad[0:1, 0:4], in_=row(b_rg_d)), "rb", writes=["rb_pad"])
        P.dma("pool", lambda e: e.dma_start(out=rb_pad[0:1, 4:36], in_=row(b_re_d)), "rb2", writes=["rb_pad"])
        P.dma("sp", lambda e: e.dma_start(out=ggla[:], in_=gla_norm_d.partition_broadcast(128)), "ggla", writes=["ggla"])
        P.dma("sp", lambda e: e.dma_start(out=gsub[:], in_=subln_d.partition_broadcast(128)), "gsub", writes=["gsub"])
        P.dve(lambda e: e.tensor_scalar(out=gsub[:], in0=gsub[:], scalar1=1.0 - LAMBDA_INIT, scalar2=None, op0=ALU.mult), reads=["gsub"], writes=["gsub"])
        for i, a in enumerate((lq1_d, lk1_d, lq2_d, lk2_d)):
            P.dma("sp", lambda e, i=i, a=a: e.dma_start(out=lqk[:, i, :], in_=a.partition_broadcast(128)), "lqk%d" % i, writes=["lqk"])
        P.dve(lambda e: e.tensor_tensor(out=lqk[:, 0, :], in0=lqk[:, 0, :], in1=lqk[:, 1, :], op=ALU.mult), reads=["lqk"], writes=["lqk"])
        P.dve(lambda e: e.tensor_tensor(out=lqk[:, 2, :], in0=lqk[:, 2, :], in1=lqk[:, 3, :], op=ALU.mult), reads=["lqk"], writes=["lqk"])
        P.dve(lambda e: e.reduce_sum(out=lam_t[:, 0:1], in_=lqk[:, 0, :], axis=AX.X), reads=["lqk"], writes=["lam"])
        P.dve(lambda e: e.reduce_sum(out=lam_t[:, 1:2], in_=lqk[:, 2, :], axis=AX.X), reads=["lqk"], writes=["lam"])
        P.act(lambda e: e.activation(out=lam_t[:, 2:4], in_=lam_t[:, 0:2], func=AF.Exp), reads=["lam"], writes=["lam"])
        P.dve(lambda e: e.tensor_tensor(out=lam_t[:, 4:5], in0=lam_t[:, 3:4], in1=lam_t[:, 2:3], op=ALU.subtract), reads=["lam"], writes=["lam"])
        P.dve(lambda e: e.tensor_scalar(out=lam_t[:, 4:5], in0=lam_t[:, 4:5], scalar1=-LAMBDA_INIT, scalar2=None, op0=ALU.add), reads=["lam"], writes=["lam"])
        neglam = lam_t[:, 4:5]

        cnt = {"nrm": 0, "pt": 0}

        def ACT(out, in_, func, reads, writes, **kw):
            P.act(lambda e: e.activation(out=out, in_=in_, func=func, **kw), reads, writes)

        def ACOPY(out, in_, reads, writes):
            P.act(lambda e: e.copy(out=out, in_=in_), reads, writes)

        def TT(eng, out, in0, in1, op, reads, writes):
            P.add(eng, lambda e: e.tensor_tensor(out=out, in0=in0, in1=in1, op=op), reads, writes)

        def TS(eng, out, in0, s1, s2, op0, op1, reads, writes):
            if s2 is None:
                P.add(eng, lambda e: e.tensor_scalar(out=out, in0=in0, scalar1=s1, scalar2=None, op0=op0), reads, writes)
            else:
                P.add(eng, lambda e: e.tensor_scalar(out=out, in0=in0, scalar1=s1, scalar2=s2, op0=op0, op1=op1), reads, writes)

        def STT(eng, out, in0, scalar, in1, op0, op1, reads, writes):
            P.add(eng, lambda e: e.scalar_tensor_tensor(out=out, in0=in0, scalar=scalar, in1=in1, op0=op0, op1=op1), reads, writes)

        def VCOPY(eng, out, in_, reads, writes):
            P.add(eng, lambda e: e.tensor_copy(out=out, in_=in_), reads, writes)

        def RSUM(out, in_, reads, writes):
            P.dve(lambda e: e.reduce_sum(out=out, in_=in_, axis=AX.X), reads, writes)

        def RMAX(out, in_, reads, writes):
            P.dve(lambda e: e.reduce_max(out=out, in_=in_, axis=AX.X), reads, writes)

        def RECIP(out, in_, reads, writes):
            P.dve(lambda e: e.reciprocal(out=out, in_=in_), reads, writes)

        def MSET(out, val, writes):
            P.pool(lambda e: e.memset(out, val), (), writes)

        def DMA(queue, out, in_, key, reads, writes, slow=False):
            if slow:
                P.dma(queue, lambda e: e.dma_start(out=out, in_=in_, allow_slow_non_contiguous=True), key, reads, writes)
            else:
                P.dma(queue, lambda e: e.dma_start(out=out, in_=in_), key, reads, writes)

        def TR(out, in_, reads, writes):
            P.pe(lambda e: e.transpose(out=out, in_=in_, identity=ident_bf[:]), list(reads) + ["ident_bf"], writes)

        def mm(out, lhsT, rhs, start, stop, reads, writes):
            P.pe(lambda e: e.matmul(out, lhsT=lhsT, rhs=rhs, start=start, stop=stop, skip_group_check=True), reads, writes)

        def rstd_ops(dst, src, scale, keys):
            ACT(dst, src, AF.Ln, keys, keys, bias=EPS, scale=scale)
            ACT(dst, dst, AF.Exp, keys, keys, scale=-0.5)

        def transpose_to(src_bf, src_key, nchunk, dst_v, dst_key, eng="act"):
            j = cnt["pt"] % 2
            cnt["pt"] += 1
            pt = PT[j]
            for c in range(nchunk):
                TR(pt[:, c * 128:(c + 1) * 128], src_bf[:, c * 128:(c + 1) * 128], [src_key], [PTK(j)])
            src_v = pt[:, 0:nchunk * 128].rearrange("p (c n) -> p c n", n=128)
            if eng == "act":
                ACOPY(dst_v, src_v, [PTK(j)], [dst_key])
            else:
                VCOPY("dve", dst_v, src_v, [PTK(j)], [dst_key])

        def norm_A(src, src_keys, gain, gain_key):
            i = cnt["nrm"] % 2
            cnt["nrm"] += 1
            junk, st, xs = NB["junk"][i], NB["st"][i], NB["xs"][i]
            MSET(st[:, 0:1], 0.0, [("st", i)])
            ACT(junk[:], src, AF.Square, list(src_keys) + [("st", i)], [("junk", i), ("st", i)], accum_out=st[:, 0:1])
            rstd_ops(st[:, 1:2], st[:, 0:1], 1.0 / D, [("st", i)])
            STT("dve", xs[:], src, st[:, 1:2], gain, ALU.mult, ALU.mult, list(src_keys) + [("st", i), gain_key], [("xs", i)])
            return i

        def norm_B(i, dst_v, dst_key):
            transpose_to(NB["xs"][i], ("xs", i), 8, dst_v, dst_key)

        def norm_T(src, src_keys, gain, gain_key, dst_v, dst_key):
            norm_B(norm_A(src, src_keys, gain, gain_key), dst_v, dst_key)

        def dbg_dump(name, ap, keys, shape, dt=F32):
            if dbg is None or name not in dbg:
                return
            d = nc.dram_tensor("dbg_" + name, list(shape), dt, kind="ExternalOutput").ap()
            dbg_outs[name] = d
            DMA("sp", d, ap, "dbg_" + name, keys, [("dbgout", name)])

        def bc_mid(ap2, n):
            return ap2.unsqueeze(1).to_broadcast([128, n, ap2.shape[1]])

        def bc_last(ap2, n):
            return ap2.unsqueeze(2).to_broadcast([128, ap2.shape[1], n])

        NB = {}

        def norm_bufs(es, tag):
            NB["xt"] = [sb("xt%s%d" % (tag, i), [128, D], F32, es) for i in range(2)]
            NB["xs"] = [sb("xs%s%d" % (tag, i), [128, D], BF16, es) for i in range(2)]
            NB["junk"] = [sb("junk%s%d" % (tag, i), [128, D], BF16, es) for i in range(2)]
            NB["st"] = [sb("st%s%d" % (tag, i), [128, 8], F32, es) for i in range(2)]

        NTK = [("nT", t) for t in range(NT)]
        OAK = [("oaT", t) for t in range(NT)]
        OBK = [("obT", q, h) for q in range(NT) for h in range(4)]
        HK = [("h", t) for t in range(NT)]

        def seq_body(s):
            first = (s == 0)
            DMA("sp", gA[:], attn_norm_d.partition_broadcast(128), "gA", [], ["gA"])
            with ExitStack() as ph:
                norm_bufs(ph, "a")
                def A1_A(t):
                    i = t % 2
                    DMA("sp", NB["xt"][i][:], x_d[s, t * 128:(t + 1) * 128, :], "xt%d" % i, [], [("xt", i)])
                    return norm_A(NB["xt"][i][:], [("xt", i)], gA[:], "gA")

                ia = A1_A(0)
                for t in range(NT):
                    ib = A1_A(t + 1) if t + 1 < NT else None
                    norm_B(ia, nT[:, :, t * 128:(t + 1) * 128], ("nT", t))
                    ia = ib
            P.barrier(scr[:, 0:1])
            if first:
                dbg_dump("nT", nT[:], NTK, [128, 8, T], BF16)
            if STOP == "A1":
                return

            DMA("sp", pos_i[:], pos_d[s].rearrange("(t p) -> p t", p=128), "pos", [], ["pos_i"], slow=True)
            VCOPY("dve", pos_f[:], pos_i[:], ["pos_i"], ["pos_f"])
            TT("dve", ang[:], bc_last(pos_f[:], 8), bc_mid(invf, NT), ALU.mult, ["pos_f", "cst"], ["ang"])
            for (dst, shift, nm) in ((sinb, 0.0, "sinb"), (cosb, math.pi / 2, "cosb")):
                TS("dve", ang2[:], ang[:], shift, None, ALU.add, None, ["ang"], ["ang2"])
                TS("dve", dst[:], ang2[:], 1.0 / (2 * math.pi), None, ALU.mult, None, ["ang2"], [nm])
                TS("dve", dst[:], dst[:], MAGIC, MAGIC, ALU.add, ALU.subtract, [nm], [nm])
                STT("dve", ang2[:], dst[:], -2 * math.pi, ang2[:], ALU.mult, ALU.add, [nm, "ang2"], ["ang2"])
                TS("dve", ang2[:], ang2[:], PI_LO, -PI_LO, ALU.min, ALU.max, ["ang2"], ["ang2"])
                ACT(dst[:], ang2[:], AF.Sin, ["ang2"], [nm])
            if first:
                dbg_dump("cosb", cosb[:], ["cosb"], [128, NT, 8])
                dbg_dump("sinb", sinb[:], ["sinb"], [128, NT, 8])
            if STOP == "rope":
                P.barrier(scr[:, 0:1])
                return

            with ExitStack() as ph:
                NPART = 2 if NG >= 2 else 1
                TP = T // NPART
                NTP = NT // NPART
                NGP = NG // NPART
                wg1 = sb("wg1", [128, 8, 1024], BF16, ph)
                wr_ = sb("wr_", [128, 8, 512], BF16, ph)
                qdec = sb("qdec", [128, 2, 2 * NTP, 128], BF16, ph)
                kinv = sb("kinv", [128, 4, TP], BF16, ph)
                kend2 = sb("kend2", [128, NTP, 2, 256], BF16, ph)
                vbf = sb("vbf", [128, NTP, 512], BF16, ph)
                ebT = sb("ebT", [128, 2, 512], F32, ph)
                einvT = sb("einvT", [128, 2, 512], F32, ph)
                decT = sb("decT", [128, 2, 2 * NTP], F32, ph)
                ed_b = [sb("ed%d" % i, [128, 256], F32, ph) for i in range(2)]
                e1_b = [sb("e1%d" % i, [128, 256], F32, ph) for i in range(2)]
                l_b = [sb("l%d" % i, [128, 256], F32, ph) for i in range(2)]
                alT = sb("alT", [128, 512], BF16, ph)
                attT_b = [sb("attT%d" % i, [128, 4, 128], BF16, ph) for i in range(2)]
                S32 = sb("S32", [128, 4, 128], F32, ph)
                Sbf_b = [sb("Sbf%d" % i, [128, 4, 128], BF16, ph) for i in range(2)]
                osb_b = [sb("osb%d" % i, [128, 512], F32, ph) for i in range(2)]
                sq = sb("sq", [128, 512], F32, ph)
                sr_all = sb("sr_all", [128, NTP, 512], F32, ph)
                oab_b = [sb("oab%d" % i, [128, 512], BF16, ph) for i in range(2)]
                gst_b = [sb("gst%d" % i, [128, 8], F32, ph) for i in range(2)]

                DMA("pool", wg1[:], cw(w_in_d[:, 0:1024]), "wg1", [], ["wg1"])
                DMA("pool", wr_[:], cw(w_in_d[:, 1024:1536]), "wr_", [], ["wr_"])
                MSET(qdec[:], 0.0, [("qdec", n) for n in range(2 * NTP)])
                MSET(kinv[:], 0.0, [("kinv", g) for g in range(NGP)])
                MSET(kend2[:], 0.0, [("kend2", t) for t in range(NTP)])
                MSET(S32[:], 0.0, ["S32"])
                MSET(Sbf_b[0][:], 0.0, [("Sbf", 0)])

                pend = [None]
                for part in range(NPART):
                    for gl in range(NGP):
                        g = part * NGP + gl
                        gs = slice(g * 512, (g + 1) * 512)
                        gsl = slice(gl * 512, (gl + 1) * 512)
                        gkeys = [("nT", 4 * g + i) for i in range(4)]
                        for c in range(8):
                            mm(BANK[4], wal_pad[:, c, :], nT[:, c, gs], c == 0, c == 7, gkeys + ["wal_pad"], [BK(4)])
                        ACOPY(alT[:], BANK[4], [BK(4)], ["alT"])
                        for tt in range(4):
                            t = 4 * g + tt
                            tl = 4 * gl + tt
                            i = t % 2
                            ts_ = slice(tt * 128, (tt + 1) * 128)
                            tsl = slice(t * 128, (t + 1) * 128)
                            zps = BANK[5][:, 0:256]
                            mm(zps, alT[:, ts_], wa2_pad[:], True, False, ["alT", "wa2_pad"], [BK(5)])
                            mm(zps, ones_bf[:], ba_pad[:], False, True, ["ones_bf", "ba_pad"], [BK(5)])
                            e1, l_, ed = e1_b[i], l_b[i], ed_b[i]
                            kps = BANK[1][:, 0:256]
                            vps = BANK[2]
                            for c in range(8):
                                mm(kps, nT[:, c, tsl], wg1[:, c, 256:512], c == 0, c == 7, [("nT", t), "wg1"], [BK(1)])
                            for c in range(8):
                                mm(vps, nT[:, c, tsl], wg1[:, c, 512:1024], c == 0, c == 7, [("nT", t), "wg1"], [BK(2)])
                            ACOPY(vbf[:, tl, :], vps, [BK(2)], [("vbf", tl)])
                            ACT(e1[:], zps, AF.Exp, [BK(5)], [("e1", i)], scale=-1.0)
                            ACT(l_[:], e1[:], AF.Ln, [("e1", i)], [("l", i)], bias=1.0)
                            b0 = BANK[0]
                            for fc in range(2):
                                mm(b0[:, fc * 128:(fc + 1) * 128], l_[:, fc * 128:(fc + 1) * 128], tri_f, True, True, [("l", i), "cst"], [BK(0)])
                            mm(b0[:, 256:512], trr_f, l_[:, :], True, True, [("l", i), "cst"], [BK(0)])
                            bTv = b0[:, 0:256].rearrange("p (f n) -> p f n", n=128)
                            ACT(ebT[:, :, ts_], bTv, AF.Exp, [BK(0)], [("ebT", tt)])
                            ACT(einvT[:, :, ts_], bTv, AF.Exp, [BK(0)], [("einvT", tt)], scale=-1.0)
                            ACT(ed[:], b0[:, 256:512], AF.Exp, [BK(0)], [("ed", i)])
                            VCOPY("pool", decT[:, :, 2 * tl:2 * tl + 2], ebT[:, :, tt * 128 + 63:tt * 128 + 128:64], [("ebT", tt)], [("decT", tl)])
                            for par in range(2):
                                ps_ = slice(par * 64, (par + 1) * 64)
                                TT("dve", kend2[ps_, tl, par, :], kps[ps_, :], ed[ps_, :], ALU.mult, [BK(1), ("ed", i)], [("kend2", tl)])
                        EBK = [("ebT", i) for i in range(4)]
                        EIK = [("einvT", i) for i in range(4)]
                        for fc in range(2):
                            qps = BANK[3]
                            for c in range(8):
                                mm(qps, wg1[:, c, fc * 128:(fc + 1) * 128], nT[:, c, gs], c == 0, c == 7, gkeys + ["wg1"], [BK(3)])
                            qv = qps.rearrange("p (t r n) -> p t r n", r=2, n=64)
                            ev = ebT[:, fc, :].rearrange("p (t r n) -> p t r n", r=2, n=64)
                            for par in range(2):
                                STT("dve", qdec[:, fc, 8 * gl + par:8 * gl + 8:2, par * 64:(par + 1) * 64], qv[:, :, par, :], 0.125, ev[:, :, par, :],
                                    ALU.mult, ALU.mult, [BK(3)] + EBK, [("qdec", 8 * gl + 2 * i + par) for i in range(4)])
                            kps2 = BANK[4]
                            for c in range(8):
                                mm(kps2, wg1[:, c, 256 + fc * 128:256 + (fc + 1) * 128], nT[:, c, gs], c == 0, c == 7, gkeys + ["wg1"], [BK(4)])
                            for hh in range(2):
                                ps_ = slice(hh * 64, (hh + 1) * 64)
                                TT("dve", kinv[ps_, 2 * fc + hh, gsl], kps2[ps_, :], einvT[ps_, fc, :], ALU.mult, [BK(4)] + EIK, [("kinv", gl)])
                    if first and part == 0:
                        dbg_dump("qdec", qdec[:], [("qdec", n) for n in range(2 * NTP)], [128, 2, 2 * NTP, 128], BF16)
                        dbg_dump("kinv", kinv[:], [("kinv", g) for g in range(NGP)], [128, 4, TP], BF16)
                        dbg_dump("kend2", kend2[:], [("kend2", t) for t in range(NTP)], [128, NTP, 2, 256], BF16)
                        dbg_dump("vbf", vbf[:], [("vbf", t) for t in range(NTP)], [128, NTP, 512], BF16)
                        dbg_dump("decT", decT[:], [("decT", t) for t in range(NTP)], [128, 2, 2 * NTP])

                    for tl in range(NTP):
                        t = part * NTP + tl
                        rb = 3 if tl % 2 == 0 else 5
                        for c in range(8):
                            mm(BANK[rb], nT[:, c, t * 128:(t + 1) * 128], wr_[:, c, :], c == 0, c == 7, [("nT", t), "wr_"], [BK(rb)])
                        ACT(sr_all[:, tl, :], BANK[rb], AF.Silu, [BK(rb)], [("sr", tl)])
                        TT("pool", sr_all[:, tl, :].rearrange("p (h n) -> p h n", n=128), sr_all[:, tl, :].rearrange("p (h n) -> p h n", n=128),
                           bc_mid(ggla[:], 4), ALU.mult, [("sr", tl), "ggla"], [("sr", tl)])
                    def ubanks(t):
                        return [BANK[0], BANK[1]], [BK(0), BK(1)]

                    def U(tl, par):
                        ups, upk = ubanks(tl)
                        for fc in range(2):
                            mm(ups[par][:, fc * 256:(fc + 1) * 256], kend2[:, tl, par, fc * 128:(fc + 1) * 128], vbf[:, tl, fc * 256:(fc + 1) * 256], True, True,
                               [("kend2", tl), ("vbf", tl)], [upk[par]])

                    def G1(tl):
                        t = part * NTP + tl
                        gl = tl // 4
                        i = t % 2
                        tll = slice(tl * 128, (tl + 1) * 128)
                        attps = BANK[4].rearrange("p (h n) -> p h n", n=128)
                        for h in range(4):
                            for par in range(2):
                                n = 2 * tl + par
                                cs = slice(par * 64, (par + 1) * 64)
                                mm(attps[:, h, cs], kinv[:, h, tll], qdec[:, h // 2, n, cs], True, True, [("kinv", gl), ("qdec", n)], [BK(4)])
                        attT = attT_b[i]
                        TT("dve", attT[:], attps, bc_mid(bd_f, 4), ALU.mult, [BK(4), "cst"], [("attT", i)])
                        ops_ = BANK[2 + i]
                        ov = ops_.rearrange("p (h n) -> p h n", n=128)
                        mm(ops_, zeros_bf[:, 0:128], zeros_bf[:, :], True, False, ["zeros_bf"], [BK(2 + i)])
                        for h in range(4):
                            mm(ov[:, h, :], attT[:, h, :], vbf[:, tl, h * 128:(h + 1) * 128], False, False, [("attT", i), ("vbf", tl)], [BK(2 + i)])

                    def G2(tl):
                        t = part * NTP + tl
                        i = t % 2
                        tsl = slice(t * 128, (t + 1) * 128)
                        ups, upk = ubanks(t)
                        ops_ = BANK[2 + i]
                        ov = ops_.rearrange("p (h n) -> p h n", n=128)
                        for par in range(2):
                            n = 2 * tl + par
                            Sb = Sbf_b[n % 2]
                            for h in range(4):
                                mm(ov[:, h, :], qdec[:, h // 2, n, :], Sb[:, h, :], False, (par == 1 and h == 3), [("qdec", n), ("Sbf", n % 2)], [BK(2 + i)])
                            for fc in range(2):
                                STT("dve", S32[:, 2 * fc:2 * fc + 2, :], S32[:, 2 * fc:2 * fc + 2, :], decT[:, fc, n:n + 1],
                                    ups[par][:, fc * 256:(fc + 1) * 256].rearrange("p (h n) -> p h n", n=128), ALU.mult, ALU.add,
                                    ["S32", ("decT", tl), upk[par]], ["S32"])
                            if tl + 1 < NTP:
                                U(tl + 1, par)
                            Sn = Sbf_b[(n + 1) % 2]
                            for hh in range(2):
                                ACT(Sn[:, hh::2, :], S32[:, hh::2, :], AF.Copy, ["S32", "cst"], [("Sbf", (n + 1) % 2)], scale=rowmask4[:, hh:hh + 1])
                        osb, gst, oab = osb_b[i], gst_b[i], oab_b[i]
                        ACOPY(osb[:], ops_, [BK(2 + i)], [("osb", i)])
                        MSET(gst[:, 0:4], 0.0, [("gst", i)])
                        for h in range(4):
                            ACT(sq[:, h * 128:(h + 1) * 128], osb[:, h * 128:(h + 1) * 128], AF.Square, [("osb", i), ("gst", i)], ["sq", ("gst", i)],
                                accum_out=gst[:, h:h + 1])
                        rstd_ops(gst[:, 4:8], gst[:, 0:4], 1.0 / 128, [("gst", i)])
                        o3 = osb[:].rearrange("p (h n) -> p h n", n=128)
                        TT("dve", o3, o3, bc_last(gst[:, 4:8], 128), ALU.mult, [("osb", i), ("gst", i)], [("osb", i)])
                        TT("dve", oab[:], osb[:], sr_all[:, tl, :], ALU.mult, [("osb", i), ("sr", tl)], [("oab", i)])
                        if pend[0] is not None:
                            transpose_to(*pend[0])
                        pend[0] = (oab, ("oab", i), 4, oaT[:, :, tsl], ("oaT", t))

                    U(0, 0)
                    U(0, 1)
                    G1(0)
                    for tl in range(NTP):
                        if tl + 1 < NTP:
                            G1(tl + 1)
                        G2(tl)
                    if part == NPART - 1:
                        transpose_to(*pend[0])
                        pend[0] = None
                if first:
                    dbg_dump("oaT", oaT[:], OAK, [128, 4, T], BF16)
            P.barrier(scr[:, 0:1])

            if STOP == "A2":
                return
            with ExitStack() as ph:
                wd_ = sb("wd_", [128, 8, 1536], BF16, ph)
                qT = sb("qT", [128, 4, T], BF16, ph)
                kTp = sb("kTp", [128, 2, 4, T], BF16, ph)
                vaug = sb("vaug", [128, NT, 4, 129], BF16, ph)
                qk_b = [sb("qk%d" % i, [128, 1024], BF16, ph) for i in range(2)]
                rt_b = [sb("rt%d" % i, [128, 4, 128], F32, ph) for i in range(2)]
                xr_b = [sb("xr%d" % i, [128, 16, 16], F32, ph) for i in range(2)]
                PTb = [sb("PTb%d" % i, [128, 512], BF16, ph) for i in range(4)]
                ob_b = [sb("ob%d" % i, [128, 128], F32, ph) for i in range(2)]
                tmp_b = [sb("tmpo%d" % i, [128, 128], F32, ph) for i in range(2)]
                obn_b = [sb("obn%d" % i, [128, 128], BF16, ph) for i in range(2)]
                dst_b = [sb("dst%d" % i, [128, 8], F32, ph) for i in range(2)]
                junk2 = sb("junk2", [128, 128], F32, ph)
                accs = sb("accs", [128, 3, 387], F32, ph)

                DMA("pool", wd_[:], cw(w_in_d[:, 1552:3088]), "wd_", [], ["wd_"])
                MSET(vaug[:], 1.0, [("vaug", t) for t in range(NT)])
                def A3_MMS(t):
                    i = t % 2
                    tsl = slice(t * 128, (t + 1) * 128)
                    qkps = PS2[i]
                    for half in range(2):
                        for c in range(8):
                            mm(qkps[:, half * 512:(half + 1) * 512], nT[:, c, tsl], wd_[:, c, half * 512:(half + 1) * 512], c == 0, c == 7,
                               [("nT", t), "wd_"], [BK(2 * i + half)])
                    vps = BANK[4 + i]
                    for c in range(8):
                        mm(vps, nT[:, c, tsl], wd_[:, c, 1024:1536], c == 0, c == 7, [("nT", t), "wd_"], [BK(4 + i)])
                    qk, xr = qk_b[i], xr_b[i]
                    QK2 = [BK(2 * i), BK(2 * i + 1)]
                    ACOPY(qk[:], qkps[:, :], QK2, [("qk", i)])
                    for half in range(2):
                        ACOPY(xr[:, half * 8:(half + 1) * 8, :],
                              qkps[:, half * 512:(half + 1) * 512].rearrange("p (a d) -> p a d", d=64)[:, :, 0:16], [BK(2 * i + half)], [("xr", i)])
                    ACOPY(vaug[:, t, :, 0:128], vps.rearrange("p (h n) -> p h n", n=128), [BK(4 + i)], [("vaug", t)])

                def A3_POST(t):
                    i = t % 2
                    tsl = slice(t * 128, (t + 1) * 128)
                    qk, rt, xr = qk_b[i], rt_b[i], xr_b[i]
                    q3 = qk[:].rearrange("p (a d) -> p a d", d=64)
                    cb = bc_mid(cosb[:, t, :], 16)
                    sn = bc_mid(sinb[:, t, :], 16)
                    r4 = rt[:].rearrange("p k (a d) -> p k a d", d=8)
                    XR = [("xr", i), "cosb", "sinb"]
                    TT("dve", r4[:, 0, :, :], xr[:, :, 0:8], cb, ALU.mult, XR, [("rt", i)])
                    TT("dve", r4[:, 1, :, :], xr[:, :, 8:16], sn, ALU.mult, XR, [("rt", i)])
                    TT("pool", r4[:, 2, :, :], xr[:, :, 8:16], cb, ALU.mult, XR, [("rt2", i)])
                    TT("pool", r4[:, 3, :, :], xr[:, :, 0:8], sn, ALU.mult, XR, [("rt2", i)])
                    TT("dve", q3[:, :, 0:8], r4[:, 0, :, :], r4[:, 1, :, :], ALU.subtract, [("rt", i), ("qk", i)], [("qk", i)])
                    TT("pool", q3[:, :, 8:16], r4[:, 2, :, :], r4[:, 3, :, :], ALU.add, [("rt2", i), ("qk", i)], [("qk", i)])
                    j = cnt["pt"] % 2
                    cnt["pt"] += 1
                    pt = PT[j]
                    for c in range(8):
                        TR(pt[:, c * 128:(c + 1) * 128], qk[:, c * 128:(c + 1) * 128], [("qk", i)], [PTK(j)])
                    pv = pt[:, :].rearrange("p (c n) -> p c n", n=128)
                    ACOPY(qT[:, :, tsl], pv[:, 0:4, :], [PTK(j)], [("qT", t)])
                    for c2 in range(2):
                        ACT(kTp[:, c2, :, tsl], pv[:, 4:8, :], AF.Copy, [PTK(j), "cst"], [("kTp", t)], scale=rowmask4[:, c2:c2 + 1])

                A3_MMS(0)
                for t in range(NT):
                    if t + 1 < NT:
                        A3_MMS(t + 1)
                    A3_POST(t)
                if first:
                    dbg_dump("qT", qT[:], [("qT", t) for t in range(NT)], [128, 4, T], BF16)
                    dbg_dump("kTp", kTp[:], [("kTp", t) for t in range(NT)], [128, 2, 4, T], BF16)
                    dbg_dump("vaug", vaug[:], [("vaug", t) for t in range(NT)], [128, NT, 4, 129], BF16)

                if STOP == "A3prep":
                    P.barrier(scr[:, 0:1])
                    return
                accb = [PS2[0][:, 0:512], PS2[0][:, 512:1024], PS2[1][:, 0:512]]

                def acc(a):
                    b, sl = divmod(a, 3)
                    return accb[b][:, sl * 129:(sl + 1) * 129], BK(b)

                steps = []
                for h in range(4):
                    for g in range(NG):
                        blk = [(h, g, j, c) for j in range(4 * g + 4) for c in range(2)]
                        for n_, st_ in enumerate(blk):
                            steps.append(st_ + (n_ == 0, n_ == len(blk) - 1))
                NS = len(steps)
                SB = [3, 4, 5]
                fin = [0]

                def geom(k):
                    h, g, j, c, _, _ = steps[k]
                    q0 = max(j, 4 * g)
                    return h, g, j, c, q0, (4 * g + 4 - q0) * 128

                def ST(k):
                    h, g, j, c, q0, ncol = geom(k)
                    bi = SB[k % 3]
                    diag = j >= 4 * g
                    mm(BANK[bi][:, 0:ncol], kTp[:, c, h, j * 128:(j + 1) * 128], qT[:, h, q0 * 128:(4 * g + 4) * 128], True, not diag,
                       [("kTp", j)] + [("qT", q) for q in range(q0, 4 * g + 4)], [BK(bi)])
                    if diag:
                        mm(BANK[bi][:, 0:128], ident_bf[:], maskneg_bf[:], False, True, ["ident_bf", "maskneg"], [BK(bi)])

                def AVs(k):
                    h, g, j, c, q0, ncol = geom(k)
                    first_, last_ = steps[k][4], steps[k][5]
                    bi = SB[k % 3]
                    pi = k % 4
                    ptb = PTb[pi]
                    if first_:
                        for b_ in range(3):
                            mm(accb[b_], zeros_bf[:, 0:128], zeros_bf[:, :], True, False, ["zeros_bf"], [BK(b_)])
                    ACT(ptb[:, 0:ncol], BANK[bi][:, 0:ncol], AF.Exp, [BK(bi)], [("PTb", pi)], scale=0.125)
                    for q in range(q0, 4 * g + 4):
                        av, ak = acc(c * 4 + (q - 4 * g))
                        mm(av, ptb[:, (q - q0) * 128:(q - q0 + 1) * 128], vaug[:, j, h, :], False, (j == q), [("PTb", pi), ("vaug", j)], [ak])
                    if last_:
                        finalize(h, g)

                def finalize(h, g):
                    for b_ in range(3):
                        VCOPY("dve", accs[:, b_, :], accb[b_][:, 0:387], [BK(b_)], [("accs", b_)])

                    def sacc(a_):
                        b_, sl = divmod(a_, 3)
                        return accs[:, b_, sl * 129:(sl + 1) * 129], ("accs", b_)

                    def fin_tile(tq):
                        q = 4 * g + tq
                        i = fin[0] % 2
                        fin[0] += 1
                        a1, k1 = sacc(tq)
                        a2, k2 = sacc(4 + tq)
                        dst, ob, tmp, obn = dst_b[i], ob_b[i], tmp_b[i], obn_b[i]
                        DK = ("dst", i)
                        RECIP(dst[:, 0:1], a1[:, 128:129], [k1], [DK])
                        RECIP(dst[:, 1:2], a2[:, 128:129], [k2], [DK])
                        TT("dve", dst[:, 1:2], dst[:, 1:2], neglam, ALU.mult, [DK, "lam"], [DK])
                        TS("dve", tmp[:], a2[:, 0:128], dst[:, 1:2], None, ALU.mult, None, [k2, DK], [("tmpo", i)])
                        STT("dve", ob[:], a1[:, 0:128], dst[:, 0:1], tmp[:], ALU.mult, ALU.add, [k1, DK, ("tmpo", i)], [("ob", i)])
                        MSET(dst[:, 2:3], 0.0, [DK])
                        ACT(junk2[:], ob[:], AF.Square, [("ob", i), DK], ["junk2", DK], accum_out=dst[:, 2:3])
                        rstd_ops(dst[:, 3:4], dst[:, 2:3], 1.0 / 128, [DK])
                        STT("dve", obn[:], ob[:], dst[:, 3:4], gsub[:], ALU.mult, ALU.mult, [("ob", i), DK, "gsub"], [("obn", i)])
                        jj = cnt["pt"] % 2
                        cnt["pt"] += 1
                        TR(PT[jj][:, 0:128], obn[:], [("obn", i)], [PTK(jj)])
                        ACOPY(obT[:, h, q * 128:(q + 1) * 128], PT[jj][:, 0:128], [PTK(jj)], [("obT", q, h)])

                    for tq in range(4):
                        deferred.append((fin_tile, tq))

                deferred = []
                for k in range(min(2, NS)):
                    ST(k)
                for k in range(NS):
                    if k + 2 < NS:
                        ST(k + 2)
                    AVs(k)
                    if deferred and not steps[k][5]:
                        f_, a_ = deferred.pop(0)
                        f_(a_)
                while deferred:
                    f_, a_ = deferred.pop(0)
                    f_(a_)
                if first:
                    dbg_dump("obT", obT[:], OBK, [128, 4, T], BF16)
            P.barrier(scr[:, 0:1])

            if STOP == "A3":
                return
            with ExitStack() as ph2:
                hm = sb("hm", [128, NT * 2 * D], BF16, ph2)
                hres = hm[:, :].bitcast(F32).rearrange("p (t d) -> p t d", d=D)
                mTv = hm[:, NT * D:2 * NT * D].rearrange("p (t c n) -> p t c n", c=8, n=128)
                with ExitStack() as ph:
                    wc_b = [sb("wc%d" % i, [128, 24, 256], BF16, ph) for i in range(2)]
                    sg_b = [sb("sg%d" % i, [128, 2, 512], F32, ph) for i in range(2)]
                    def load_wc(jb):
                        wi_ = jb % 2
                        wcb = wc_b[wi_]
                        c2 = slice(jb * 256, (jb + 1) * 256)
                        DMA("pool", wcb[:, 0:8, :], cw(w_in_d[:, 3088 + jb * 256:3088 + (jb + 1) * 256]), "wc%d" % wi_, [], [("wc", wi_)])
                        DMA("pool", wcb[:, 8:16, :], cw(w_in_d[:, 4112 + jb * 256:4112 + (jb + 1) * 256]), "wc%d" % wi_, [], [("wc", wi_)])
                        DMA("pool", wcb[:, 16:20, :], cw(w_ba_d[:, c2]), "wc%d" % wi_, [], [("wc", wi_)])
                        DMA("pool", wcb[:, 20:24, :], cw(w_bb_d[:, c2]), "wc%d" % wi_, [], [("wc", wi_)])

                    load_wc(0)
                    for j in range(8):
                        jb = j // 2
                        wi = jb % 2
                        if j % 2 == 0 and jb + 1 < 4:
                            load_wc(jb + 1)
                        wc = wc_b[wi][:, :, (j % 2) * 128:(j % 2 + 1) * 128]
                        WCK = ("wc", wi)
                        for g in range(NG):
                            gs = slice(g * 512, (g + 1) * 512)
                            it = j * NG + g
                            ii = it % 2
                            bsel = [(4 * it + r) % 6 for r in range(4)]
                            gkeys = [("nT", 4 * g + i) for i in range(4)]
                            for c in range(4):
                                mm(BANK[bsel[0]], wc[:, 16 + c, :], oaT[:, c, gs], c == 0, c == 3, OAK + [WCK], [BK(bsel[0])])
                            for c in range(4):
                                mm(BANK[bsel[1]], wc[:, 20 + c, :], obT[:, c, gs], c == 0, c == 3, OBK + [WCK], [BK(bsel[1])])
                            for c in range(8):
                                mm(BANK[bsel[2]], wc[:, c, :], nT[:, c, gs], c == 0, c == 7, gkeys + [WCK], [BK(bsel[2])])
                            for c in range(8):
                                mm(BANK[bsel[3]], wc[:, 8 + c, :], nT[:, c, gs], c == 0, c == 7, gkeys + [WCK], [BK(bsel[3])])
                            sg = sg_b[ii]
                            ACT(sg[:, 0, :], BANK[bsel[2]], AF.Sigmoid, [BK(bsel[2])], [("sg", ii, 0)])
                            ACT(sg[:, 1, :], BANK[bsel[3]], AF.Sigmoid, [BK(bsel[3])], [("sg", ii, 1)])
                            TT("dve", sg[:, 0, :], BANK[bsel[0]], sg[:, 0, :], ALU.mult, [BK(bsel[0]), ("sg", ii, 0)], [("sg", ii, 0)])
                            TT("dve", sg[:, 1, :], BANK[bsel[1]], sg[:, 1, :], ALU.mult, [BK(bsel[1]), ("sg", ii, 1)], [("sg", ii, 1)])
                            TT("pool", mTv[:, 4 * g:4 * g + 4, j, :], sg[:, 0, :].rearrange("p (t n) -> p t n", n=128),
                               sg[:, 1, :].rearrange("p (t n) -> p t n", n=128), ALU.add,
                               [("sg", ii, 0), ("sg", ii, 1)], [("mT", 4 * g + i) for i in range(4)])
                    if first:
                        dbg_dump("mT", mTv, [("mT", t) for t in range(NT)], [128, NT, 8, 128], BF16)
                P.barrier(scr[:, 0:1])
                if STOP == "C1":
                    return
                with ExitStack() as ph:
                    wo = sb("wo", [128, 8, D], BF16, ph)
                    rl_all = sb("rl_all", [128, NT, 36], F32, ph)
                    rs = sb("rs", [128, 8, NT], F32, ph)
                    r4a = sb("r4a", [128, 2, NT, 4], F32, ph)
                    r8 = sb("r8", [128, 6, NT, 8], F32, ph)
                    norm_bufs(ph, "c")
                    DMA("pool", wo[:], cw(w_out_d), "wo", [], ["wo"])
                    DMA("sp", gB[:], ffn_norm_d.partition_broadcast(128), "gB", [], ["gB"])
                    def HM(t):
                        i = t % 2
                        xt = NB["xt"][i]
                        DMA("sp", xt[:], x_d[s, t * 128:(t + 1) * 128, :], "xt%d" % i, [], [("xt", i)])
                        hps = PS2[i]
                        H2 = [BK(2 * i), BK(2 * i + 1)]
                        for half in range(2):
                            for c in range(8):
                                mm(hps[:, half * 512:(half + 1) * 512], mTv[:, t, c, :], wo[:, c, half * 512:(half + 1) * 512], c == 0, c == 7,
                                   [("mT", t), "wo"], [BK(2 * i + half)])
                        alias = []
                        if t >= NT // 2:
                            alias = [("mT", 2 * (t - NT // 2)), ("mT", 2 * (t - NT // 2) + 1)]
                        TT("dve", hres[:, t, :], hps[:, :], xt[:], ALU.add, H2 + [("xt", i)], [("h", t)] + alias)

                    HM(0)
                    for t in range(NT):
                        i = t % 2
                        tsl = slice(t * 128, (t + 1) * 128)
                        ix = norm_A(hres[:, t, :], [("h", t)], gB[:], "gB")
                        if t + 1 < NT:
                            HM(t + 1)
                        norm_B(ix, nT[:, :, tsl], ("nT", t))
                        rps = BANK[4 + i][:, 0:36]
                        for c in range(8):
                            mm(rps, nT[:, c, tsl], wr_bf[:, c, :], c == 0, False, [("nT", t), "wr_bf"], [BK(4 + i)])
                        mm(rps, ones_bf[:], rb_pad[:], False, True, ["ones_bf", "rb_pad"], [BK(4 + i)])
                        ACOPY(rl_all[:, t, :], rps, [BK(4 + i)], [("rl", t)])
                    RL = [("rl", t) for t in range(NT)]
                    RW = ["rw"]
                    gl = rl_all[:, :, 0:4]
                    el4 = rl_all[:, :, 4:36].rearrange("p t (g e) -> p t g e", e=8)

                    def bl(ap2, n):
                        return ap2.unsqueeze(2).to_broadcast([128, NT, n])

                    gmax, gsum, gp, emax, m2, ed_, w1, w2 = [rs[:, i_, :] for i_ in range(8)]
                    selg, gexp = r4a[:, 0, :, :], r4a[:, 1, :, :]
                    el, oh1, el2, oh2, cwt, tmp8 = [r8[:, i_, :, :] for i_ in range(6)]
                    RMAX(gmax, gl, RL, RW)
                    TT("dve", selg, gl, bl(gmax, 4), ALU.is_ge, RL + RW, RW)
                    TT("dve", gexp, gl, bl(gmax, 4), ALU.subtract, RL + RW, RW)
                    ACT(gexp, gexp, AF.Exp, RW, RW)
                    RSUM(gsum, gexp, RW, RW)
                    RECIP(gp, gsum, RW, RW)
                    TT("dve", el, el4[:, :, 0, :], bl(selg[:, :, 0], 8), ALU.mult, RL + RW, RW)
                    for g_ in range(1, 4):
                        TT("dve", tmp8, el4[:, :, g_, :], bl(selg[:, :, g_], 8), ALU.mult, RL + RW, RW)
                        TT("dve", el, el, tmp8, ALU.add, RW, RW)
                    RMAX(emax, el, RW, RW)
                    TT("dve", oh1, el, bl(emax, 8), ALU.is_ge, RW, RW)
                    STT("dve", el2, oh1, -1e30, el, ALU.mult, ALU.add, RW, RW)
                    RMAX(m2, el2, RW, RW)
                    TT("dve", oh2, el2, bl(m2, 8), ALU.is_ge, RW, RW)
                    TT("dve", ed_, emax, m2, ALU.subtract, RW, RW)
                    ACT(ed_, ed_, AF.Exp, RW, RW)
                    TS("dve", ed_, ed_, 1.0, None, ALU.add, None, RW, RW)
                    RECIP(w2, ed_, RW, RW)
                    TS("dve", w1, w2, -1.0, 1.0, ALU.mult, ALU.add, RW, RW)
                    TT("dve", w1, w1, gp, ALU.mult, RW, RW)
                    TT("dve", w2, w2, gp, ALU.mult, RW, RW)
                    TT("dve", cwt, oh1, bl(w1, 8), ALU.mult, RW, RW)
                    TT("dve", tmp8, oh2, bl(w2, 8), ALU.mult, RW, RW)
                    TT("dve", cwt, cwt, tmp8, ALU.add, RW, RW)
                    comb4 = comb[:, :, :].rearrange("p t (g e) -> p t g e", e=8)
                    for g_ in range(4):
                        TT("dve", comb4[:, :, g_, :], cwt, bl(selg[:, :, g_], 8), ALU.mult, RW, [("comb", t) for t in range(NT)])
                    if first:
                        dbg_dump("h1", hres, HK, [128, NT, D])
                        dbg_dump("comb", comb[:], [("comb", t) for t in range(NT)], [128, NT, 32])
                        dbg_dump("n2T", nT[:], NTK, [128, 8, T], BF16)
                P.barrier(scr[:, 0:1])

                if STOP == "C2":
                    return
                with ExitStack() as ph:
                    wgu_b = [sb("wgu%d" % i, [128, 8, 512], BF16, ph) for i in range(3)]
                    wdn_b = [sb("wdn%d" % i, [128, 2, D], BF16, ph) for i in range(3)]
                    sgl_b = [sb("sgl%d" % i, [128, 256], F32, ph) for i in range(2)]
                    he_b = [sb("he%d" % i, [128, 256], BF16, ph) for i in range(2)]
                    heT_b = [sb("heT%d" % i, [128, 2, 128], BF16, ph) for i in range(2)]
                    units = [(ex, t) for ex in range(NEX) for t in range(NT)]
                    NU = len(units)

                    def load_w(ex):
                        wi = ex % 3
                        DMA("pool", wgu_b[wi][:, :, 0:256], cw(w_gate_d[ex]), "wgu%d" % wi, [], [("wgu", wi)])
                        DMA("pool", wgu_b[wi][:, :, 256:512], cw(w_up_d[ex]), "wgu%d" % wi, [], [("wgu", wi)])
                        DMA("pool", wdn_b[wi][:], cw(w_down_d[ex]), "wdn%d" % wi, [], [("wdn", wi)])

                    def S1(u):
                        ex, t = units[u]
                        i, wi = u % 2, ex % 3
                        if t == 2 and ex + 2 < NEX:
                            load_w(ex + 2)
                        gups = BANK[4 + i]
                        for c in range(8):
                            mm(gups, nT[:, c, t * 128:(t + 1) * 128], wgu_b[wi][:, c, :], c == 0, c == 7, [("nT", t), ("wgu", wi)], [BK(4 + i)])
                        ACT(sgl_b[i][:], gups[:, 0:256], AF.Silu, [BK(4 + i)], [("sgl", i)])
                        STT("dve", he_b[i][:], gups[:, 256:512], comb[:, t, ex:ex + 1], sgl_b[i][:], ALU.mult, ALU.mult,
                            [BK(4 + i), ("comb", t), ("sgl", i)], [("he", i)])

                    def S2(u):
                        i = u % 2
                        pt = PT[i]
                        for c in range(2):
                            TR(pt[:, c * 128:(c + 1) * 128], he_b[i][:, c * 128:(c + 1) * 128], [("he", i)], [PTK(i)])
                        ACOPY(heT_b[i][:], pt[:, 0:256].rearrange("p (c n) -> p c n", n=128), [PTK(i)], [("heT", i)])

                    def S3(u):
                        ex, t = units[u]
                        i, wi = u % 2, ex % 3
                        yps = PS2[i]
                        for half in range(2):
                            for c in range(2):
                                mm(yps[:, half * 512:(half + 1) * 512], heT_b[i][:, c, :], wdn_b[wi][:, c, half * 512:(half + 1) * 512], c == 0, c == 1,
                                   [("heT", i), ("wdn", wi)], [BK(2 * i + half)])
                        TT("dve", hres[:, t, :], yps[:, :], hres[:, t, :], ALU.add, [BK(2 * i), BK(2 * i + 1), ("h", t)], [("h", t)])

                    for ex in range(min(2, NEX)):
                        load_w(ex)
                    for k in range(NU + 2):
                        if k < NU:
                            S1(k)
                        if 0 <= k - 1 < NU:
                            S2(k - 1)
                        if 0 <= k - 2 < NU:
                            S3(k - 2)
                    if first:
                        dbg_dump("h2", hres, HK, [128, NT, D])
                P.barrier(scr[:, 0:1])

                if STOP == "D":
                    return
                with ExitStack() as ph:
                    wpg = sb("wpg", [128, 8, D], BF16, ph)
                    wple = sb("wple", [128, 2, D], BF16, ph)
                    pt_b = [sb("ptile%d" % i, [128, 256], F32, ph) for i in range(3)]
                    pb_b = [sb("pb%d" % i, [128, 256], BF16, ph) for i in range(2)]
                    pT_b = [sb("pT%d" % i, [128, 2, 128], BF16, ph) for i in range(2)]
                    hb_b = [sb("hb%d" % i, [128, D], BF16, ph) for i in range(2)]
                    hT_b = [sb("hT%d" % i, [128, 8, 128], BF16, ph) for i in range(2)]
                    en_b = [sb("en%d" % i, [128, D], F32, ph) for i in range(1)]
                    sgt_b = [sb("sgt%d" % i, [128, D], F32, ph) for i in range(2)]
                    ot_b = [sb("ot%d" % i, [128, D], F32, ph) for i in range(1)]
                    junk_e = sb("junk_e", [128, D], BF16, ph)
                    est_b = [sb("est%d" % i, [128, 8], F32, ph) for i in range(2)]
                    DMA("pool", wpg[:], cw(w_pg_d), "wpg", [], ["wpg"])
                    DMA("pool", wple[:], cw(w_ple_d), "wple", [], ["wple"])
                    DMA("sp", gA[:], ple_norm_d.partition_broadcast(128), "gA", [], ["gA"])
                    DMA("sp", gB[:], final_norm_d.partition_broadcast(128), "gB", [], ["gB"])
                    def E1a(t):
                        i = t % 2
                        VCOPY("dve", pb_b[i][:], pt_b[t % 3][:], [("ptile", t % 3)], [("pb", i)])
                        VCOPY("dve", hb_b[i][:], hres[:, t, :], [("h", t)], [("hb", i)])

                    def E1b(t):
                        i = t % 2
                        transpose_to(pb_b[i], ("pb", i), 2, pT_b[i][:, :, :], ("pT", i))
                        transpose_to(hb_b[i], ("hb", i), 8, hT_b[i][:, :, :], ("hT", i))

                    def E2(t):
                        i = t % 2
                        pT, hT, en, sgt, est = pT_b[i], hT_b[i], en_b[0], sgt_b[i], est_b[i]
                        EK, SG = ("est", i), ("sgt", i)
                        eps_ = PS2[0]
                        for half in range(2):
                            for c in range(2):
                                mm(eps_[:, half * 512:(half + 1) * 512], pT[:, c, :], wple[:, c, half * 512:(half + 1) * 512], c == 0, c == 1, [("pT", i), "wple"], [BK(half)])
                        gps = PS2[1]
                        for half in range(2):
                            for c in range(8):
                                mm(gps[:, half * 512:(half + 1) * 512], hT[:, c, :], wpg[:, c, half * 512:(half + 1) * 512], c == 0, c == 7, [("hT", i), "wpg"], [BK(2 + half)])
                        MSET(est[:, 0:4], 0.0, [EK])
                        ACT(junk_e[:], eps_[:, :], AF.Square, [BK(0), BK(1), EK], ["junk_e", EK], accum_out=est[:, 0:1])
                        rstd_ops(est[:, 1:2], est[:, 0:1], 1.0 / D, [EK])
                        STT("dve", en[:], eps_[:, :], est[:, 1:2], gA[:], ALU.mult, ALU.mult, [BK(0), BK(1), EK, "gA"], [("en", 0)])
                        ACT(sgt[:], gps[:, :], AF.Sigmoid, [BK(2), BK(3)], [SG])
                        TT("dve", sgt[:], sgt[:], en[:], ALU.mult, [SG, ("en", 0)], [SG])
                        TT("pool", sgt[:], sgt[:], hres[:, t, :], ALU.add, [SG, ("h", t)], [SG])

                    def E3(t):
                        i = t % 2
                        sgt, ot, est = sgt_b[i], ot_b[0], est_b[i]
                        EK, SG = ("est", i), ("sgt", i)
                        ACT(junk_e[:], sgt[:], AF.Square, [SG, EK], ["junk_e", EK], accum_out=est[:, 2:3])
                        rstd_ops(est[:, 3:4], est[:, 2:3], 1.0 / D, [EK])
                        STT("dve", ot[:], sgt[:], est[:, 3:4], gB[:], ALU.mult, ALU.mult, [SG, EK, "gB"], [("ot", 0)])
                        DMA("sp", out_d[s, t * 128:(t + 1) * 128, :], ot[:], "out0", [("ot", 0)], [("out", 0)])

                    def E0(t):
                        DMA("pool", pt_b[t % 3][:], p_d[s, t * 128:(t + 1) * 128, :], "ptile%d" % (t % 3), [], [("ptile", t % 3)])

                    E0(0)
                    if NT > 1:
                        E0(1)
                    E1a(0)
                    E1b(0)
                    for k in range(NT + 1):
                        if k + 2 < NT:
                            E0(k + 2)
                        if k >= 1:
                            E3(k - 1)
                        if k + 1 < NT:
                            E1a(k + 1)
                        if k < NT:
                            E2(k)
                        if k + 1 < NT:
                            E1b(k + 1)
            P.barrier(scr[:, 0:1])

        for s in range(NSEQ):
            seq_body(s)

        fk = [("out", 0)] + [("dbgout", n) for n in dbg_outs]
        counts = P.emit(fk)
    build.info = {"nops": len(P.ops), "signals": counts, "dbg": list(dbg_outs)}
    return nc


_CACHE = {}


def _weights_map(inp):
    g = lambda k: np.ascontiguousarray(np.asarray(inp[k]))
    return {
        "attn_norm": g("attn_norm")[0], "w_in": g("w_in")[0], "w_a2": g("w_a2")[0], "b_a": g("b_a")[0],
        "gla_norm": g("gla_norm")[0], "lq1": g("lambda_q1")[0], "lk1": g("lambda_k1")[0],
        "lq2": g("lambda_q2")[0], "lk2": g("lambda_k2")[0], "diff_subln": g("diff_subln")[0],
        "w_ba": g("w_branch_a")[0], "w_bb": g("w_branch_b")[0], "w_out": g("w_out")[0],
        "ffn_norm": g("ffn_norm")[0], "w_rg": g("w_router_group")[0], "b_rg": g("b_router_group")[0],
        "w_re": g("w_router_expert")[0], "b_re": g("b_router_expert")[0],
        "w_gate": g("w_gate")[0], "w_up": g("w_up")[0], "w_down": g("w_down")[0],
        "w_ple": g("w_ple")[0], "ple_norm": g("ple_norm")[0], "w_pg": g("w_ple_gate")[0],
        "final_norm": g("final_norm"), "cst": make_consts(),
    }


def kernel(**inputs):
    x = np.asarray(inputs["x"], dtype=np.float32)
    p = np.asarray(inputs["p"], dtype=np.float32)[0]
    pos = np.asarray(inputs["positions"], dtype=np.int32)
    B, T, _ = x.shape
    ncores = 8
    nseq = B // ncores
    key = (T, nseq)
    if key not in _CACHE:
        _CACHE[key] = build(T=T, NSEQ=nseq)
    nc = _CACHE[key]
    w = _weights_map(inputs)
    in_maps = []
    for c in range(ncores):
        m = dict(w)
        m["x"] = np.ascontiguousarray(x[c * nseq:(c + 1) * nseq])
        m["p"] = np.ascontiguousarray(p[c * nseq:(c + 1) * nseq])
        m["pos"] = np.ascontiguousarray(pos[c * nseq:(c + 1) * nseq])
        in_maps.append(m)
    res = run_bass_kernel_spmd(nc, in_maps, core_ids=list(range(ncores)))
    out = np.concatenate([np.asarray(r["out"]) for r in res.results], axis=0)
    return out.astype(np.float32, copy=False)
```

```python
import math
import os
from contextlib import ExitStack

import numpy as np
import concourse.bass as bass
import concourse.mybir as mybir
from concourse.bass_utils import run_bass_kernel_spmd

F32 = mybir.dt.float32
BF16 = mybir.dt.bfloat16
I32 = mybir.dt.int32
ALU = mybir.AluOpType
AF = mybir.ActivationFunctionType
AX = mybir.AxisListType

D = 1024
DIN = 5136
NE = 32
EPS = 1e-6
LAMBDA_INIT = 0.8 - 0.6 * math.exp(-0.3 * 0)
MAGIC = 12582912.0
PI_LO = 3.1415925
NCST = 656
SK = os.environ.get("KSKIP", "")

COMPUTE = ("pe", "act", "dve", "pool")
SAME_ENGINE_SYNC = {"pe": False, "act": True, "dve": True, "pool": True, "sp": False}


class Op:
    __slots__ = ("eng", "fn", "reads", "writes", "dma", "waits", "dma_waits",
                 "signal", "token", "dma_val")

    def __init__(self, eng, fn, reads, writes, dma):
        self.eng = eng
        self.fn = fn
        self.reads = reads
        self.writes = writes
        self.dma = dma
        self.waits = []
        self.dma_waits = []
        self.signal = False
        self.token = 0
        self.dma_val = 0


class Prog:
    def __init__(self, nc):
        self.nc = nc
        self.ops = []
        self.last_writer = {}
        self.readers = {}
        self.dma_count = {}
        self.known = {e: {} for e in ("pe", "act", "dve", "pool", "sp")}
        self.snap = []

    def add(self, eng, fn, reads=(), writes=(), dma=None):
        i = len(self.ops)
        op = Op(eng, fn, tuple(reads), tuple(writes), dma)
        deps = set()
        for k in op.reads:
            w = self.last_writer.get(k)
            if w is not None:
                deps.add(w)
        for k in op.writes:
            w = self.last_writer.get(k)
            if w is not None:
                deps.add(w)
            r = self.readers.get(k)
            if r:
                deps.update(r)
        known = self.known[eng]
        need = {}
        for j in deps:
            pj = self.ops[j]
            if pj.dma is not None:
                key = ("dma", pj.dma)
                if known.get(key, 0) < pj.dma_val and need.get(key, 0) < pj.dma_val:
                    need[key] = pj.dma_val
            else:
                if pj.eng == eng and not SAME_ENGINE_SYNC[eng]:
                    continue
                if known.get(pj.eng, -1) < j and need.get(pj.eng, -1) < j:
                    need[pj.eng] = j
        for key, v in need.items():
            if isinstance(key, tuple):
                op.dma_waits.append((key[1], v))
                known[key] = v
            else:
                op.waits.append(v)
                self.ops[v].signal = True
                known[key] = v
                for kk, vv in self.snap[v].items():
                    if known.get(kk, -1) < vv:
                        known[kk] = vv
        if dma is not None:
            self.dma_count[dma] = self.dma_count.get(dma, 0) + 16
            op.dma_val = self.dma_count[dma]
        self.snap.append(dict(known))
        for k in op.reads:
            self.readers.setdefault(k, []).append(i)
        for k in op.writes:
            self.last_writer[k] = i
            self.readers[k] = []
        self.ops.append(op)
        return i

    def pe(self, fn, reads=(), writes=()):
        return self.add("pe", fn, reads, writes)

    def act(self, fn, reads=(), writes=()):
        return self.add("act", fn, reads, writes)

    def dve(self, fn, reads=(), writes=()):
        return self.add("dve", fn, reads, writes)

    def pool(self, fn, reads=(), writes=()):
        return self.add("pool", fn, reads, writes)

    def dma(self, queue, fn, key, reads=(), writes=()):
        return self.add(queue, fn, reads, writes, dma=key)

    def barrier(self, scratch):
        keys = set(self.last_writer) | set(self.readers)
        keys.add("__bar")
        self.add("pool", lambda e: e.memset(scratch, 0.0), reads=(), writes=tuple(keys))
        for eng in ("pe", "act", "dve", "sp"):
            self.add(eng, None, reads=("__bar",))
        self.last_writer = {"__bar": self.last_writer["__bar"]}
        self.readers = {}

    def emit(self, final_keys):
        nc = self.nc
        self.add("sp", None, reads=tuple(final_keys), writes=())
        counts = {e: 0 for e in self.known}
        for op in self.ops:
            if op.signal:
                counts[op.eng] += 1
                op.token = counts[op.eng]
        with ExitStack() as es:
            sems = {e: es.enter_context(nc.semaphore("s_" + e)) for e in COMPUTE}
            dsems = {}
            for k in self.dma_count:
                dsems[k] = es.enter_context(nc.semaphore("d_%d" % len(dsems)))
            block = es.enter_context(nc.Block())
            ops = self.ops

            def run(eng_name):
                def body(e):
                    for op in ops:
                        if op.eng != eng_name:
                            continue
                        for j in op.waits:
                            pj = ops[j]
                            e.wait_ge(sems[pj.eng], pj.token)
                        for (k, v) in op.dma_waits:
                            e.wait_ge(dsems[k], v)
                        if op.fn is None:
                            continue
                        ins = op.fn(e)
                        if op.dma is not None:
                            ins.then_inc(dsems[op.dma], 16)
                        elif op.signal:
                            ins.then_inc(sems[op.eng], 1)
                return body

            block.tensor(run("pe"))
            block.scalar(run("act"))
            block.vector(run("dve"))
            block.gpsimd(run("pool"))
            block.sync(run("sp"))
        return counts


def make_consts():
    c = np.zeros((128, NCST), np.float32)
    i = np.arange(128)
    c[:, 0:128] = np.eye(128, dtype=np.float32)
    c[:, 128:256] = (i[:, None] <= i[None, :]).astype(np.float32)
    same = (i[:, None] // 64) == (i[None, :] // 64)
    bd = (same & (i[:, None] <= i[None, :])).astype(np.float32)
    c[:, 256:384] = bd
    c[:, 384:512] = -bd / 16.0
    c[:, 512:640] = -(same & (i[:, None] > i[None, :])).astype(np.float32) / 16.0
    inv = 500000.0 ** (-np.arange(0, 16, 2, dtype=np.float32) / 16.0)
    c[:, 640:648] = inv.astype(np.float32)[None, :]
    for h in range(4):
        c[:, 648 + h] = ((i // 64) == (h % 2)).astype(np.float32)
    return c


def build(T=2048, NSEQ=2, dbg=None, NEX=NE, STOP=None):
    NT = T // 128
    NG = T // 512
    nc = bass.Bass("TRN2", target_bir_lowering=False)

    def din(name, shape, dt=F32):
        return nc.dram_tensor(name, list(shape), dt, kind="ExternalInput").ap()

    x_d = din("x", [NSEQ, T, D])
    p_d = din("p", [NSEQ, T, 256])
    pos_d = din("pos", [NSEQ, T], I32)
    attn_norm_d = din("attn_norm", [D])
    w_in_d = din("w_in", [D, DIN])
    w_a2_d = din("w_a2", [16, 256])
    b_a_d = din("b_a", [256])
    gla_norm_d = din("gla_norm", [128])
    lq1_d = din("lq1", [64]); lk1_d = din("lk1", [64]); lq2_d = din("lq2", [64]); lk2_d = din("lk2", [64])
    subln_d = din("diff_subln", [128])
    w_ba_d = din("w_ba", [512, D])
    w_bb_d = din("w_bb", [512, D])
    w_out_d = din("w_out", [D, D])
    ffn_norm_d = din("ffn_norm", [D])
    w_rg_d = din("w_rg", [D, 4]); b_rg_d = din("b_rg", [4])
    w_re_d = din("w_re", [D, 32]); b_re_d = din("b_re", [32])
    w_gate_d = din("w_gate", [NE, D, 256])
    w_up_d = din("w_up", [NE, D, 256])
    w_down_d = din("w_down", [NE, 256, D])
    w_ple_d = din("w_ple", [256, D])
    ple_norm_d = din("ple_norm", [D])
    w_pg_d = din("w_pg", [D, D])
    final_norm_d = din("final_norm", [D])
    cst_d = din("cst", [128, NCST])
    out_d = nc.dram_tensor("out", [NSEQ, T, D], F32, kind="ExternalOutput").ap()
    dbg_outs = {}

    P = Prog(nc)
    top = ExitStack()

    nsb = [0]

    def sb(name, shape, dt, es=None):
        nsb[0] += 1
        return (es or top).enter_context(nc.sbuf_tensor("sb%d_%s" % (nsb[0], name), list(shape), dt))

    def cw(ap):
        return ap.rearrange("(c p) n -> p c n", p=128)

    def row(ap):
        return ap.rearrange("(o n) -> o n", o=1)

    with top:
        PS2 = [top.enter_context(nc.psum_tensor("ps2_%d" % i, [128, 1024], F32)) for i in range(2)]
        PS1 = [top.enter_context(nc.psum_tensor("ps1_%d" % i, [128, 512], F32)) for i in range(2)]
        PT = [top.enter_context(nc.psum_tensor("pst_%d" % i, [128, 1024], BF16)) for i in range(2)]
        BANK = [PS2[0][:, 0:512], PS2[0][:, 512:1024], PS2[1][:, 0:512], PS2[1][:, 512:1024],
                PS1[0][:, :], PS1[1][:, :]]

        def BK(i):
            return ("B", i)

        def PTK(i):
            return ("PT", i)

        cst = sb("cst", [128, NCST], F32)
        ident_f = cst[:, 0:128]
        caus_f = cst[:, 128:256]
        bd_f = cst[:, 256:384]
        tri_f = cst[:, 384:512]
        trr_f = cst[:, 512:640]
        invf = cst[:, 640:648]
        rowmask4 = cst[:, 648:652]
        ident_bf = sb("ident_bf", [128, 128], BF16)
        caus_bf = sb("caus_bf", [128, 128], BF16)
        ones_bf = sb("ones_bf", [128, 128], BF16)
        maskneg_bf = sb("maskneg_bf", [128, 128], BF16)
        zeros_bf = sb("zeros_bf", [128, 512], BF16)
        scr = sb("scr", [128, 4], F32)
        wal_pad = sb("wal_pad", [128, 8, 128], BF16)
        wa2_pad = sb("wa2_pad", [128, 256], BF16)
        ba_pad = sb("ba_pad", [128, 256], BF16)
        wr_bf = sb("wr_bf", [128, 8, 36], BF16)
        rb_pad = sb("rb_pad", [128, 36], BF16)
        ggla = sb("ggla", [128, 128], F32)
        gsub = sb("gsub", [128, 128], F32)
        lqk = sb("lqk", [128, 4, 64], F32)
        lam_t = sb("lam_t", [128, 8], F32)
        gA = sb("gA", [128, D], F32)
        gB = sb("gB", [128, D], F32)
        nT = sb("nT", [128, 8, T], BF16)
        oaT = sb("oaT", [128, 4, T], BF16)
        obT = sb("obT", [128, 4, T], BF16)
        cosb = sb("cosb", [128, NT, 8], F32)
        sinb = sb("sinb", [128, NT, 8], F32)
        pos_i = sb("pos_i", [128, NT], I32)
        pos_f = sb("pos_f", [128, NT], F32)
        ang = sb("ang", [128, NT, 8], F32)
        ang2 = sb("ang2", [128, NT, 8], F32)
        comb = sb("comb", [128, NT, 32], F32)

        P.dma("sp", lambda e: e.dma_start(out=cst[:], in_=cst_d), "cst", writes=["cst"])
        P.dve(lambda e: e.tensor_copy(out=ident_bf[:], in_=ident_f), reads=["cst"], writes=["ident_bf"])
        P.dve(lambda e: e.tensor_copy(out=caus_bf[:], in_=caus_f), reads=["cst"], writes=["caus_bf"])
        P.pool(lambda e: e.memset(ones_bf[:], 1.0), writes=["ones_bf"])
        P.dve(lambda e: e.tensor_scalar(out=maskneg_bf[:], in0=caus_f, scalar1=-1.0, scalar2=30000.0, op0=ALU.add, op1=ALU.mult), reads=["cst"], writes=["maskneg"])
        P.pool(lambda e: e.memset(zeros_bf[:], 0.0), writes=["zeros_bf"])
        P.pool(lambda e: e.memset(wal_pad[:], 0.0), writes=["wal_pad"])
        P.pool(lambda e: e.memset(wa2_pad[:], 0.0), writes=["wa2_pad"])
        P.pool(lambda e: e.memset(ba_pad[:], 0.0), writes=["ba_pad"])
        P.pool(lambda e: e.memset(rb_pad[:], 0.0), writes=["rb_pad"])
        P.dma("pool", lambda e: e.dma_start(out=wal_pad[:, :, 0:16], in_=cw(w_in_d[:, 1536:1552])), "wal", writes=["wal_pad"])
        P.dma("pool", lambda e: e.dma_start(out=wa2_pad[0:16, :], in_=w_a2_d), "wa2", writes=["wa2_pad"])
        P.dma("pool", lambda e: e.dma_start(out=ba_pad[0:1, :], in_=row(b_a_d)), "ba", writes=["ba_pad"])
        P.dma("pool", lambda e: e.dma_start(out=wr_bf[:, :, 0:4], in_=cw(w_rg_d)), "wr", writes=["wr_bf"])
        P.dma("pool", lambda e: e.dma_start(out=wr_bf[:, :, 4:36], in_=cw(w_re_d)), "wr2", writes=["wr_bf"])
        P.dma("pool", lambda e: e.dma_start(out=rb_pad[0:1, 0:4], in_=row(b_rg_d)), "rb", writes=["rb_pad"])
        P.dma("pool", lambda e: e.dma_start(out=rb_pad[0:1, 4:36], in_=row(b_re_d)), "rb2", writes=["rb_pad"])
        P.dma("sp", lambda e: e.dma_start(out=ggla[:], in_=gla_norm_d.partition_broadcast(128)), "ggla", writes=["ggla"])
        P.dma("sp", lambda e: e.dma_start(out=gsub[:], in_=subln_d.partition_broadcast(128)), "gsub", writes=["gsub"])
        P.dve(lambda e: e.tensor_scalar(out=gsub[:], in0=gsub[:], scalar1=1.0 - LAMBDA_INIT, scalar2=None, op0=ALU.mult), reads=["gsub"], writes=["gsub"])
        for i, a in enumerate((lq1_d, lk1_d, lq2_d, lk2_d)):
            P.dma("sp", lambda e, i=i, a=a: e.dma_start(out=lqk[:, i, :], in_=a.partition_broadcast(128)), "lqk%d" % i, writes=["lqk"])
        P.dve(lambda e: e.tensor_tensor(out=lqk[:, 0, :], in0=lqk[:, 0, :], in1=lqk[:, 1, :], op=ALU.mult), reads=["lqk"], writes=["lqk"])
        P.dve(lambda e: e.tensor_tensor(out=lqk[:, 2, :], in0=lqk[:, 2, :], in1=lqk[:, 3, :], op=ALU.mult), reads=["lqk"], writes=["lqk"])
        P.dve(lambda e: e.reduce_sum(out=lam_t[:, 0:1], in_=lqk[:, 0, :], axis=AX.X), reads=["lqk"], writes=["lam"])
        P.dve(lambda e: e.reduce_sum(out=lam_t[:, 1:2], in_=lqk[:, 2, :], axis=AX.X), reads=["lqk"], writes=["lam"])
        P.act(lambda e: e.activation(out=lam_t[:, 2:4], in_=lam_t[:, 0:2], func=AF.Exp), reads=["lam"], writes=["lam"])
        P.dve(lambda e: e.tensor_tensor(out=lam_t[:, 4:5], in0=lam_t[:, 3:4], in1=lam_t[:, 2:3], op=ALU.subtract), reads=["lam"], writes=["lam"])
        P.dve(lambda e: e.tensor_scalar(out=lam_t[:, 4:5], in0=lam_t[:, 4:5], scalar1=-LAMBDA_INIT, scalar2=None, op0=ALU.add), reads=["lam"], writes=["lam"])
        neglam = lam_t[:, 4:5]

        cnt = {"nrm": 0, "pt": 0}

        def ACT(out, in_, func, reads, writes, **kw):
            P.act(lambda e: e.activation(out=out, in_=in_, func=func, **kw), reads, writes)

        def ACOPY(out, in_, reads, writes):
            P.act(lambda e: e.copy(out=out, in_=in_), reads, writes)

        def TT(eng, out, in0, in1, op, reads, writes):
            P.add(eng, lambda e: e.tensor_tensor(out=out, in0=in0, in1=in1, op=op), reads, writes)

        def TS(eng, out, in0, s1, s2, op0, op1, reads, writes):
            if s2 is None:
                P.add(eng, lambda e: e.tensor_scalar(out=out, in0=in0, scalar1=s1, scalar2=None, op0=op0), reads, writes)
            else:
                P.add(eng, lambda e: e.tensor_scalar(out=out, in0=in0, scalar1=s1, scalar2=s2, op0=op0, op1=op1), reads, writes)

        def STT(eng, out, in0, scalar, in1, op0, op1, reads, writes):
            P.add(eng, lambda e: e.scalar_tensor_tensor(out=out, in0=in0, scalar=scalar, in1=in1, op0=op0, op1=op1), reads, writes)

        def VCOPY(eng, out, in_, reads, writes):
            P.add(eng, lambda e: e.tensor_copy(out=out, in_=in_), reads, writes)

        def RSUM(out, in_, reads, writes):
            P.dve(lambda e: e.reduce_sum(out=out, in_=in_, axis=AX.X), reads, writes)

        def RMAX(out, in_, reads, writes):
            P.dve(lambda e: e.reduce_max(out=out, in_=in_, axis=AX.X), reads, writes)

        def RECIP(out, in_, reads, writes):
            P.dve(lambda e: e.reciprocal(out=out, in_=in_), reads, writes)

        def MSET(out, val, writes):
            P.pool(lambda e: e.memset(out, val), (), writes)

        def DMA(queue, out, in_, key, reads, writes, slow=False):
            if slow:
                P.dma(queue, lambda e: e.dma_start(out=out, in_=in_, allow_slow_non_contiguous=True), key, reads, writes)
            else:
                P.dma(queue, lambda e: e.dma_start(out=out, in_=in_), key, reads, writes)

        def TR(out, in_, reads, writes):
            P.pe(lambda e: e.transpose(out=out, in_=in_, identity=ident_bf[:]), list(reads) + ["ident_bf"], writes)

        def mm(out, lhsT, rhs, start, stop, reads, writes):
            P.pe(lambda e: e.matmul(out, lhsT=lhsT, rhs=rhs, start=start, stop=stop, skip_group_check=True), reads, writes)

        def rstd_ops(dst, src, scale, keys):
            ACT(dst, src, AF.Ln, keys, keys, bias=EPS, scale=scale)
            ACT(dst, dst, AF.Exp, keys, keys, scale=-0.5)

        def transpose_to(src_bf, src_key, nchunk, dst_v, dst_key, eng="act"):
            j = cnt["pt"] % 2
            cnt["pt"] += 1
            pt = PT[j]
            for c in range(nchunk):
                TR(pt[:, c * 128:(c + 1) * 128], src_bf[:, c * 128:(c + 1) * 128], [src_key], [PTK(j)])
            src_v = pt[:, 0:nchunk * 128].rearrange("p (c n) -> p c n", n=128)
            if eng == "act":
                ACOPY(dst_v, src_v, [PTK(j)], [dst_key])
            else:
                VCOPY("dve", dst_v, src_v, [PTK(j)], [dst_key])

        def norm_A(src, src_keys, gain, gain_key):
            i = cnt["nrm"] % 2
            cnt["nrm"] += 1
            junk, st, xs = NB["junk"][i], NB["st"][i], NB["xs"][i]
            MSET(st[:, 0:1], 0.0, [("st", i)])
            ACT(junk[:], src, AF.Square, list(src_keys) + [("st", i)], [("junk", i), ("st", i)], accum_out=st[:, 0:1])
            rstd_ops(st[:, 1:2], st[:, 0:1], 1.0 / D, [("st", i)])
            STT("dve", xs[:], src, st[:, 1:2], gain, ALU.mult, ALU.mult, list(src_keys) + [("st", i), gain_key], [("xs", i)])
            return i

        def norm_B(i, dst_v, dst_key):
            transpose_to(NB["xs"][i], ("xs", i), 8, dst_v, dst_key)

        def norm_T(src, src_keys, gain, gain_key, dst_v, dst_key):
            norm_B(norm_A(src, src_keys, gain, gain_key), dst_v, dst_key)

        def dbg_dump(name, ap, keys, shape, dt=F32):
            if dbg is None or name not in dbg:
                return
            d = nc.dram_tensor("dbg_" + name, list(shape), dt, kind="ExternalOutput").ap()
            dbg_outs[name] = d
            DMA("sp", d, ap, "dbg_" + name, keys, [("dbgout", name)])

        def bc_mid(ap2, n):
            return ap2.unsqueeze(1).to_broadcast([128, n, ap2.shape[1]])

        def bc_last(ap2, n):
            return ap2.unsqueeze(2).to_broadcast([128, ap2.shape[1], n])

        NB = {}

        def norm_bufs(es, tag):
            NB["xt"] = [sb("xt%s%d" % (tag, i), [128, D], F32, es) for i in range(2)]
            NB["xs"] = [sb("xs%s%d" % (tag, i), [128, D], BF16, es) for i in range(2)]
            NB["junk"] = [sb("junk%s%d" % (tag, i), [128, D], BF16, es) for i in range(2)]
            NB["st"] = [sb("st%s%d" % (tag, i), [128, 8], F32, es) for i in range(2)]

        NTK = [("nT", t) for t in range(NT)]
        OAK = [("oaT", t) for t in range(NT)]
        OBK = [("obT", q, h) for q in range(NT) for h in range(4)]
        HK = [("h", t) for t in range(NT)]

        def seq_body(s):
            first = (s == 0)
            DMA("sp", gA[:], attn_norm_d.partition_broadcast(128), "gA", [], ["gA"])
            with ExitStack() as ph:
                norm_bufs(ph, "a")
                def A1_A(t):
                    i = t % 2
                    DMA("sp", NB["xt"][i][:], x_d[s, t * 128:(t + 1) * 128, :], "xt%d" % i, [], [("xt", i)])
                    return norm_A(NB["xt"][i][:], [("xt", i)], gA[:], "gA")

                ia = A1_A(0)
                for t in range(NT):
                    ib = A1_A(t + 1) if t + 1 < NT else None
                    norm_B(ia, nT[:, :, t * 128:(t + 1) * 128], ("nT", t))
                    ia = ib
            P.barrier(scr[:, 0:1])
            if first:
                dbg_dump("nT", nT[:], NTK, [128, 8, T], BF16)
            if STOP == "A1":
                return

            DMA("sp", pos_i[:], pos_d[s].rearrange("(t p) -> p t", p=128), "pos", [], ["pos_i"], slow=True)
            VCOPY("dve", pos_f[:], pos_i[:], ["pos_i"], ["pos_f"])
            TT("dve", ang[:], bc_last(pos_f[:], 8), bc_mid(invf, NT), ALU.mult, ["pos_f", "cst"], ["ang"])
            for (dst, shift, nm) in ((sinb, 0.0, "sinb"), (cosb, math.pi / 2, "cosb")):
                TS("dve", ang2[:], ang[:], shift, None, ALU.add, None, ["ang"], ["ang2"])
                TS("dve", dst[:], ang2[:], 1.0 / (2 * math.pi), None, ALU.mult, None, ["ang2"], [nm])
                TS("dve", dst[:], dst[:], MAGIC, MAGIC, ALU.add, ALU.subtract, [nm], [nm])
                STT("dve", ang2[:], dst[:], -2 * math.pi, ang2[:], ALU.mult, ALU.add, [nm, "ang2"], ["ang2"])
                TS("dve", ang2[:], ang2[:], PI_LO, -PI_LO, ALU.min, ALU.max, ["ang2"], ["ang2"])
                ACT(dst[:], ang2[:], AF.Sin, ["ang2"], [nm])
            if first:
                dbg_dump("cosb", cosb[:], ["cosb"], [128, NT, 8])
                dbg_dump("sinb", sinb[:], ["sinb"], [128, NT, 8])
            if STOP == "rope":
                P.barrier(scr[:, 0:1])
                return

            with ExitStack() as ph:
                NPART = 2 if NG >= 2 else 1
                TP = T // NPART
                NTP = NT // NPART
                NGP = NG // NPART
                wg1 = sb("wg1", [128, 8, 1024], BF16, ph)
                wr_ = sb("wr_", [128, 8, 512], BF16, ph)
                qdec = sb("qdec", [128, 2, 2 * NTP, 128], BF16, ph)
                kinv = sb("kinv", [128, 4, TP], BF16, ph)
                kend2 = sb("kend2", [128, NTP, 2, 256], BF16, ph)
                vbf = sb("vbf", [128, NTP, 512], BF16, ph)
                ebT = sb("ebT", [128, 2, 512], F32, ph)
                einvT = sb("einvT", [128, 2, 512], F32, ph)
                decT = sb("decT", [128, 2, 2 * NTP], F32, ph)
                ed_b = [sb("ed%d" % i, [128, 256], F32, ph) for i in range(2)]
                e1_b = [sb("e1%d" % i, [128, 256], F32, ph) for i in range(2)]
                l_b = [sb("l%d" % i, [128, 256], F32, ph) for i in range(2)]
                alT = sb("alT", [128, 512], BF16, ph)
                attT_b = [sb("attT%d" % i, [128, 4, 128], BF16, ph) for i in range(2)]
                S32 = sb("S32", [128, 4, 128], F32, ph)
                Sbf_b = [sb("Sbf%d" % i, [128, 4, 128], BF16, ph) for i in range(2)]
                osb_b = [sb("osb%d" % i, [128, 512], F32, ph) for i in range(2)]
                sq = sb("sq", [128, 512], F32, ph)
                sr_all = sb("sr_all", [128, NTP, 512], F32, ph)
                oab_b = [sb("oab%d" % i, [128, 512], BF16, ph) for i in range(2)]
                gst_b = [sb("gst%d" % i, [128, 8], F32, ph) for i in range(2)]

                DMA("pool", wg1[:], cw(w_in_d[:, 0:1024]), "wg1", [], ["wg1"])
                DMA("pool", wr_[:], cw(w_in_d[:, 1024:1536]), "wr_", [], ["wr_"])
                MSET(qdec[:], 0.0, [("qdec", n) for n in range(2 * NTP)])
                MSET(kinv[:], 0.0, [("kinv", g) for g in range(NGP)])
                MSET(kend2[:], 0.0, [("kend2", t) for t in range(NTP)])
                MSET(S32[:], 0.0, ["S32"])
                MSET(Sbf_b[0][:], 0.0, [("Sbf", 0)])

                pend = [None]
                for part in range(NPART):
                    for gl in range(NGP):
                        g = part * NGP + gl
                        gs = slice(g * 512, (g + 1) * 512)
                        gsl = slice(gl * 512, (gl + 1) * 512)
                        gkeys = [("nT", 4 * g + i) for i in range(4)]
                        for c in range(8):
                            mm(BANK[4], wal_pad[:, c, :], nT[:, c, gs], c == 0, c == 7, gkeys + ["wal_pad"], [BK(4)])
                        ACOPY(alT[:], BANK[4], [BK(4)], ["alT"])
                        for tt in range(4):
                            t = 4 * g + tt
                            tl = 4 * gl + tt
                            i = t % 2
                            ts_ = slice(tt * 128, (tt + 1) * 128)
                            tsl = slice(t * 128, (t + 1) * 128)
                            zps = BANK[5][:, 0:256]
                            mm(zps, alT[:, ts_], wa2_pad[:], True, False, ["alT", "wa2_pad"], [BK(5)])
                            mm(zps, ones_bf[:], ba_pad[:], False, True, ["ones_bf", "ba_pad"], [BK(5)])
                            e1, l_, ed = e1_b[i], l_b[i], ed_b[i]
                            kps = BANK[1][:, 0:256]
                            vps = BANK[2]
                            for c in range(8):
                                mm(kps, nT[:, c, tsl], wg1[:, c, 256:512], c == 0, c == 7, [("nT", t), "wg1"], [BK(1)])
                            for c in range(8):
                                mm(vps, nT[:, c, tsl], wg1[:, c, 512:1024], c == 0, c == 7, [("nT", t), "wg1"], [BK(2)])
                            ACOPY(vbf[:, tl, :], vps, [BK(2)], [("vbf", tl)])
                            ACT(e1[:], zps, AF.Exp, [BK(5)], [("e1", i)], scale=-1.0)
                            ACT(l_[:], e1[:], AF.Ln, [("e1", i)], [("l", i)], bias=1.0)
                            b0 = BANK[0]
                            for fc in range(2):
                                mm(b0[:, fc * 128:(fc + 1) * 128], l_[:, fc * 128:(fc + 1) * 128], tri_f, True, True, [("l", i), "cst"], [BK(0)])
                            mm(b0[:, 256:512], trr_f, l_[:, :], True, True, [("l", i), "cst"], [BK(0)])
                            bTv = b0[:, 0:256].rearrange("p (f n) -> p f n", n=128)
                            ACT(ebT[:, :, ts_], bTv, AF.Exp, [BK(0)], [("ebT", tt)])
                            ACT(einvT[:, :, ts_], bTv, AF.Exp, [BK(0)], [("einvT", tt)], scale=-1.0)
                            ACT(ed[:], b0[:, 256:512], AF.Exp, [BK(0)], [("ed", i)])
                            VCOPY("pool", decT[:, :, 2 * tl:2 * tl + 2], ebT[:, :, tt * 128 + 63:tt * 128 + 128:64], [("ebT", tt)], [("decT", tl)])
                            for par in range(2):
                                ps_ = slice(par * 64, (par + 1) * 64)
                                TT("dve", kend2[ps_, tl, par, :], kps[ps_, :], ed[ps_, :], ALU.mult, [BK(1), ("ed", i)], [("kend2", tl)])
                        EBK = [("ebT", i) for i in range(4)]
                        EIK = [("einvT", i) for i in range(4)]
                        for fc in range(2):
                            qps = BANK[3]
                            for c in range(8):
                                mm(qps, wg1[:, c, fc * 128:(fc + 1) * 128], nT[:, c, gs], c == 0, c == 7, gkeys + ["wg1"], [BK(3)])
                            qv = qps.rearrange("p (t r n) -> p t r n", r=2, n=64)
                            ev = ebT[:, fc, :].rearrange("p (t r n) -> p t r n", r=2, n=64)
                            for par in range(2):
                                STT("dve", qdec[:, fc, 8 * gl + par:8 * gl + 8:2, par * 64:(par + 1) * 64], qv[:, :, par, :], 0.125, ev[:, :, par, :],
                                    ALU.mult, ALU.mult, [BK(3)] + EBK, [("qdec", 8 * gl + 2 * i + par) for i in range(4)])
                            kps2 = BANK[4]
                            for c in range(8):
                                mm(kps2, wg1[:, c, 256 + fc * 128:256 + (fc + 1) * 128], nT[:, c, gs], c == 0, c == 7, gkeys + ["wg1"], [BK(4)])
                            for hh in range(2):
                                ps_ = slice(hh * 64, (hh + 1) * 64)
                                TT("dve", kinv[ps_, 2 * fc + hh, gsl], kps2[ps_, :], einvT[ps_, fc, :], ALU.mult, [BK(4)] + EIK, [("kinv", gl)])
                    if first and part == 0:
                        dbg_dump("qdec", qdec[:], [("qdec", n) for n in range(2 * NTP)], [128, 2, 2 * NTP, 128], BF16)
                        dbg_dump("kinv", kinv[:], [("kinv", g) for g in range(NGP)], [128, 4, TP], BF16)
                        dbg_dump("kend2", kend2[:], [("kend2", t) for t in range(NTP)], [128, NTP, 2, 256], BF16)
                        dbg_dump("vbf", vbf[:], [("vbf", t) for t in range(NTP)], [128, NTP, 512], BF16)
                        dbg_dump("decT", decT[:], [("decT", t) for t in range(NTP)], [128, 2, 2 * NTP])

                    for tl in range(NTP):
                        t = part * NTP + tl
                        rb = 3 if tl % 2 == 0 else 5
                        for c in range(8):
                            mm(BANK[rb], nT[:, c, t * 128:(t + 1) * 128], wr_[:, c, :], c == 0, c == 7, [("nT", t), "wr_"], [BK(rb)])
                        ACT(sr_all[:, tl, :], BANK[rb], AF.Silu, [BK(rb)], [("sr", tl)])
                        TT("pool", sr_all[:, tl, :].rearrange("p (h n) -> p h n", n=128), sr_all[:, tl, :].rearrange("p (h n) -> p h n", n=128),
                           bc_mid(ggla[:], 4), ALU.mult, [("sr", tl), "ggla"], [("sr", tl)])
                    def ubanks(t):
                        return [BANK[0], BANK[1]], [BK(0), BK(1)]

                    def U(tl, par):
                        ups, upk = ubanks(tl)
                        for fc in range(2):
                            mm(ups[par][:, fc * 256:(fc + 1) * 256], kend2[:, tl, par, fc * 128:(fc + 1) * 128], vbf[:, tl, fc * 256:(fc + 1) * 256], True, True,
                               [("kend2", tl), ("vbf", tl)], [upk[par]])

                    def G1(tl):
                        t = part * NTP + tl
                        gl = tl // 4
                        i = t % 2
                        tll = slice(tl * 128, (tl + 1) * 128)
                        attps = BANK[4].rearrange("p (h n) -> p h n", n=128)
                        for h in range(4):
                            for par in range(2):
                                n = 2 * tl + par
                                cs = slice(par * 64, (par + 1) * 64)
                                mm(attps[:, h, cs], kinv[:, h, tll], qdec[:, h // 2, n, cs], True, True, [("kinv", gl), ("qdec", n)], [BK(4)])
                        attT = attT_b[i]
                        TT("dve", attT[:], attps, bc_mid(bd_f, 4), ALU.mult, [BK(4), "cst"], [("attT", i)])
                        ops_ = BANK[2 + i]
                        ov = ops_.rearrange("p (h n) -> p h n", n=128)
                        mm(ops_, zeros_bf[:, 0:128], zeros_bf[:, :], True, False, ["zeros_bf"], [BK(2 + i)])
                        for h in range(4):
                            mm(ov[:, h, :], attT[:, h, :], vbf[:, tl, h * 128:(h + 1) * 128], False, False, [("attT", i), ("vbf", tl)], [BK(2 + i)])

                    def G2(tl):
                        t = part * NTP + tl
                        i = t % 2
                        tsl = slice(t * 128, (t + 1) * 128)
                        ups, upk = ubanks(t)
                        ops_ = BANK[2 + i]
                        ov = ops_.rearrange("p (h n) -> p h n", n=128)
                        for par in range(2):
                            n = 2 * tl + par
                            Sb = Sbf_b[n % 2]
                            for h in range(4):
                                mm(ov[:, h, :], qdec[:, h // 2, n, :], Sb[:, h, :], False, (par == 1 and h == 3), [("qdec", n), ("Sbf", n % 2)], [BK(2 + i)])
                            for fc in range(2):
                                STT("dve", S32[:, 2 * fc:2 * fc + 2, :], S32[:, 2 * fc:2 * fc + 2, :], decT[:, fc, n:n + 1],
                                    ups[par][:, fc * 256:(fc + 1) * 256].rearrange("p (h n) -> p h n", n=128), ALU.mult, ALU.add,
                                    ["S32", ("decT", tl), upk[par]], ["S32"])
                            if tl + 1 < NTP:
                                U(tl + 1, par)
                            Sn = Sbf_b[(n + 1) % 2]
                            for hh in range(2):
                                ACT(Sn[:, hh::2, :], S32[:, hh::2, :], AF.Copy, ["S32", "cst"], [("Sbf", (n + 1) % 2)], scale=rowmask4[:, hh:hh + 1])
                        osb, gst, oab = osb_b[i], gst_b[i], oab_b[i]
                        ACOPY(osb[:], ops_, [BK(2 + i)], [("osb", i)])
                        MSET(gst[:, 0:4], 0.0, [("gst", i)])
                        for h in range(4):
                            ACT(sq[:, h * 128:(h + 1) * 128], osb[:, h * 128:(h + 1) * 128], AF.Square, [("osb", i), ("gst", i)], ["sq", ("gst", i)],
                                accum_out=gst[:, h:h + 1])
                        rstd_ops(gst[:, 4:8], gst[:, 0:4], 1.0 / 128, [("gst", i)])
                        o3 = osb[:].rearrange("p (h n) -> p h n", n=128)
                        TT("dve", o3, o3, bc_last(gst[:, 4:8], 128), ALU.mult, [("osb", i), ("gst", i)], [("osb", i)])
                        TT("dve", oab[:], osb[:], sr_all[:, tl, :], ALU.mult, [("osb", i), ("sr", tl)], [("oab", i)])
                        if pend[0] is not None:
                            transpose_to(*pend[0])
                        pend[0] = (oab, ("oab", i), 4, oaT[:, :, tsl], ("oaT", t))

                    U(0, 0)
                    U(0, 1)
                    G1(0)
                    for tl in range(NTP):
                        if tl + 1 < NTP:
                            G1(tl + 1)
                        G2(tl)
                    if part == NPART - 1:
                        transpose_to(*pend[0])
                        pend[0] = None
                if first:
                    dbg_dump("oaT", oaT[:], OAK, [128, 4, T], BF16)
            P.barrier(scr[:, 0:1])

            if STOP == "A2":
                return
            with ExitStack() as ph:
                wd_ = sb("wd_", [128, 8, 1536], BF16, ph)
                qT = sb("qT", [128, 4, T], BF16, ph)
                kTp = sb("kTp", [128, 2, 4, T], BF16, ph)
                vaug = sb("vaug", [128, NT, 4, 129], BF16, ph)
                qk_b = [sb("qk%d" % i, [128, 1024], BF16, ph) for i in range(2)]
                rt_b = [sb("rt%d" % i, [128, 4, 128], F32, ph) for i in range(2)]
                xr_b = [sb("xr%d" % i, [128, 16, 16], F32, ph) for i in range(2)]
                PTb = [sb("PTb%d" % i, [128, 512], BF16, ph) for i in range(4)]
                ob_b = [sb("ob%d" % i, [128, 128], F32, ph) for i in range(2)]
                tmp_b = [sb("tmpo%d" % i, [128, 128], F32, ph) for i in range(2)]
                obn_b = [sb("obn%d" % i, [128, 128], BF16, ph) for i in range(2)]
                dst_b = [sb("dst%d" % i, [128, 8], F32, ph) for i in range(2)]
                junk2 = sb("junk2", [128, 128], F32, ph)
                accs = sb("accs", [128, 3, 387], F32, ph)

                DMA("pool", wd_[:], cw(w_in_d[:, 1552:3088]), "wd_", [], ["wd_"])
                MSET(vaug[:], 1.0, [("vaug", t) for t in range(NT)])
                def A3_MMS(t):
                    i = t % 2
                    tsl = slice(t * 128, (t + 1) * 128)
                    qkps = PS2[i]
                    for half in range(2):
                        for c in range(8):
                            mm(qkps[:, half * 512:(half + 1) * 512], nT[:, c, tsl], wd_[:, c, half * 512:(half + 1) * 512], c == 0, c == 7,
                               [("nT", t), "wd_"], [BK(2 * i + half)])
                    vps = BANK[4 + i]
                    for c in range(8):
                        mm(vps, nT[:, c, tsl], wd_[:, c, 1024:1536], c == 0, c == 7, [("nT", t), "wd_"], [BK(4 + i)])
                    qk, xr = qk_b[i], xr_b[i]
                    QK2 = [BK(2 * i), BK(2 * i + 1)]
                    ACOPY(qk[:], qkps[:, :], QK2, [("qk", i)])
                    for half in range(2):
                        ACOPY(xr[:, half * 8:(half + 1) * 8, :],
                              qkps[:, half * 512:(half + 1) * 512].rearrange("p (a d) -> p a d", d=64)[:, :, 0:16], [BK(2 * i + half)], [("xr", i)])
                    ACOPY(vaug[:, t, :, 0:128], vps.rearrange("p (h n) -> p h n", n=128), [BK(4 + i)], [("vaug", t)])

                def A3_POST(t):
                    i = t % 2
                    tsl = slice(t * 128, (t + 1) * 128)
                    qk, rt, xr = qk_b[i], rt_b[i], xr_b[i]
                    q3 = qk[:].rearrange("p (a d) -> p a d", d=64)
                    cb = bc_mid(cosb[:, t, :], 16)
                    sn = bc_mid(sinb[:, t, :], 16)
                    r4 = rt[:].rearrange("p k (a d) -> p k a d", d=8)
                    XR = [("xr", i), "cosb", "sinb"]
                    TT("dve", r4[:, 0, :, :], xr[:, :, 0:8], cb, ALU.mult, XR, [("rt", i)])
                    TT("dve", r4[:, 1, :, :], xr[:, :, 8:16], sn, ALU.mult, XR, [("rt", i)])
                    TT("pool", r4[:, 2, :, :], xr[:, :, 8:16], cb, ALU.mult, XR, [("rt2", i)])
                    TT("pool", r4[:, 3, :, :], xr[:, :, 0:8], sn, ALU.mult, XR, [("rt2", i)])
                    TT("dve", q3[:, :, 0:8], r4[:, 0, :, :], r4[:, 1, :, :], ALU.subtract, [("rt", i), ("qk", i)], [("qk", i)])
                    TT("pool", q3[:, :, 8:16], r4[:, 2, :, :], r4[:, 3, :, :], ALU.add, [("rt2", i), ("qk", i)], [("qk", i)])
                    j = cnt["pt"] % 2
                    cnt["pt"] += 1
                    pt = PT[j]
                    for c in range(8):
                        TR(pt[:, c * 128:(c + 1) * 128], qk[:, c * 128:(c + 1) * 128], [("qk", i)], [PTK(j)])
                    pv = pt[:, :].rearrange("p (c n) -> p c n", n=128)
                    ACOPY(qT[:, :, tsl], pv[:, 0:4, :], [PTK(j)], [("qT", t)])
                    for c2 in range(2):
                        ACT(kTp[:, c2, :, tsl], pv[:, 4:8, :], AF.Copy, [PTK(j), "cst"], [("kTp", t)], scale=rowmask4[:, c2:c2 + 1])

                A3_MMS(0)
                for t in range(NT):
                    if t + 1 < NT:
                        A3_MMS(t + 1)
                    A3_POST(t)
                if first:
                    dbg_dump("qT", qT[:], [("qT", t) for t in range(NT)], [128, 4, T], BF16)
                    dbg_dump("kTp", kTp[:], [("kTp", t) for t in range(NT)], [128, 2, 4, T], BF16)
                    dbg_dump("vaug", vaug[:], [("vaug", t) for t in range(NT)], [128, NT, 4, 129], BF16)

                if STOP == "A3prep":
                    P.barrier(scr[:, 0:1])
                    return
                accb = [PS2[0][:, 0:512], PS2[0][:, 512:1024], PS2[1][:, 0:512]]

                def acc(a):
                    b, sl = divmod(a, 3)
                    return accb[b][:, sl * 129:(sl + 1) * 129], BK(b)

                steps = []
                for h in range(4):
                    for g in range(NG):
                        blk = [(h, g, j, c) for j in range(4 * g + 4) for c in range(2)]
                        for n_, st_ in enumerate(blk):
                            steps.append(st_ + (n_ == 0, n_ == len(blk) - 1))
                NS = len(steps)
                SB = [3, 4, 5]
                fin = [0]

                def geom(k):
                    h, g, j, c, _, _ = steps[k]
                    q0 = max(j, 4 * g)
                    return h, g, j, c, q0, (4 * g + 4 - q0) * 128

                def ST(k):
                    h, g, j, c, q0, ncol = geom(k)
                    bi = SB[k % 3]
                    diag = j >= 4 * g
                    mm(BANK[bi][:, 0:ncol], kTp[:, c, h, j * 128:(j + 1) * 128], qT[:, h, q0 * 128:(4 * g + 4) * 128], True, not diag,
                       [("kTp", j)] + [("qT", q) for q in range(q0, 4 * g + 4)], [BK(bi)])
                    if diag:
                        mm(BANK[bi][:, 0:128], ident_bf[:], maskneg_bf[:], False, True, ["ident_bf", "maskneg"], [BK(bi)])

                def AVs(k):
                    h, g, j, c, q0, ncol = geom(k)
                    first_, last_ = steps[k][4], steps[k][5]
                    bi = SB[k % 3]
                    pi = k % 4
                    ptb = PTb[pi]
                    if first_:
                        for b_ in range(3):
                            mm(accb[b_], zeros_bf[:, 0:128], zeros_bf[:, :], True, False, ["zeros_bf"], [BK(b_)])
                    ACT(ptb[:, 0:ncol], BANK[bi][:, 0:ncol], AF.Exp, [BK(bi)], [("PTb", pi)], scale=0.125)
                    for q in range(q0, 4 * g + 4):
                        av, ak = acc(c * 4 + (q - 4 * g))
                        mm(av, ptb[:, (q - q0) * 128:(q - q0 + 1) * 128], vaug[:, j, h, :], False, (j == q), [("PTb", pi), ("vaug", j)], [ak])
                    if last_:
                        finalize(h, g)

                def finalize(h, g):
                    for b_ in range(3):
                        VCOPY("dve", accs[:, b_, :], accb[b_][:, 0:387], [BK(b_)], [("accs", b_)])

                    def sacc(a_):
                        b_, sl = divmod(a_, 3)
                        return accs[:, b_, sl * 129:(sl + 1) * 129], ("accs", b_)

                    def fin_a(tq):
                        i = (fin[0] + tq) % 2
                        a1, k1 = sacc(tq)
                        a2, k2 = sacc(4 + tq)
                        dst, ob, tmp, obn = dst_b[i], ob_b[i], tmp_b[i], obn_b[i]
                        DK = ("dst", i)
                        RECIP(dst[:, 0:1], a1[:, 128:129], [k1], [DK])
                        RECIP(dst[:, 1:2], a2[:, 128:129], [k2], [DK])
                        TT("dve", dst[:, 1:2], dst[:, 1:2], neglam, ALU.mult, [DK, "lam"], [DK])
                        TS("dve", tmp[:], a2[:, 0:128], dst[:, 1:2], None, ALU.mult, None, [k2, DK], [("tmpo", i)])
                        STT("dve", ob[:], a1[:, 0:128], dst[:, 0:1], tmp[:], ALU.mult, ALU.add, [k1, DK, ("tmpo", i)], [("ob", i)])
                        MSET(dst[:, 2:3], 0.0, [DK])

                    def fin_b(tq):
                        q = 4 * g + tq
                        i = (fin[0] + tq) % 2
                        dst, ob, obn = dst_b[i], ob_b[i], obn_b[i]
                        DK = ("dst", i)
                        ACT(junk2[:], ob[:], AF.Square, [("ob", i), DK], ["junk2", DK], accum_out=dst[:, 2:3])
                        rstd_ops(dst[:, 3:4], dst[:, 2:3], 1.0 / 128, [DK])
                        STT("dve", obn[:], ob[:], dst[:, 3:4], gsub[:], ALU.mult, ALU.mult, [("ob", i), DK, "gsub"], [("obn", i)])
                        jj = cnt["pt"] % 2
                        cnt["pt"] += 1
                        TR(PT[jj][:, 0:128], obn[:], [("obn", i)], [PTK(jj)])
                        ACOPY(obT[:, h, q * 128:(q + 1) * 128], PT[jj][:, 0:128], [PTK(jj)], [("obT", q, h)])

                    deferred.append([(fin_a, 0)])
                    deferred.append([(fin_b, 0), (fin_a, 1)])
                    deferred.append([(fin_b, 1), (fin_a, 2)])
                    deferred.append([(fin_b, 2), (fin_a, 3)])
                    deferred.append([(fin_b, 3)])

                deferred = []
                for k in range(min(2, NS)):
                    ST(k)
                for k in range(NS):
                    if k + 2 < NS:
                        ST(k + 2)
                    AVs(k)
                    if deferred and not steps[k][5]:
                        for f_, a_ in deferred.pop(0):
                            f_(a_)
                while deferred:
                    for f_, a_ in deferred.pop(0):
                        f_(a_)
                if first:
                    dbg_dump("obT", obT[:], OBK, [128, 4, T], BF16)
            P.barrier(scr[:, 0:1])

            if STOP == "A3":
                return
            with ExitStack() as ph2:
                hm = sb("hm", [128, NT * 2 * D], BF16, ph2)
                hres = hm[:, :].bitcast(F32).rearrange("p (t d) -> p t d", d=D)
                mTv = hm[:, NT * D:2 * NT * D].rearrange("p (t c n) -> p t c n", c=8, n=128)
                with ExitStack() as ph:
                    wc_b = [sb("wc%d" % i, [128, 24, 256], BF16, ph) for i in range(2)]
                    sg_b = [sb("sg%d" % i, [128, 2, 512], F32, ph) for i in range(2)]
                    def load_wc(jb):
                        wi_ = jb % 2
                        wcb = wc_b[wi_]
                        c2 = slice(jb * 256, (jb + 1) * 256)
                        DMA("pool", wcb[:, 0:8, :], cw(w_in_d[:, 3088 + jb * 256:3088 + (jb + 1) * 256]), "wc%d" % wi_, [], [("wc", wi_)])
                        DMA("pool", wcb[:, 8:16, :], cw(w_in_d[:, 4112 + jb * 256:4112 + (jb + 1) * 256]), "wc%d" % wi_, [], [("wc", wi_)])
                        DMA("pool", wcb[:, 16:20, :], cw(w_ba_d[:, c2]), "wc%d" % wi_, [], [("wc", wi_)])
                        DMA("pool", wcb[:, 20:24, :], cw(w_bb_d[:, c2]), "wc%d" % wi_, [], [("wc", wi_)])

                    load_wc(0)
                    for j in range(8):
                        jb = j // 2
                        wi = jb % 2
                        if j % 2 == 0 and jb + 1 < 4:
                            load_wc(jb + 1)
                        wc = wc_b[wi][:, :, (j % 2) * 128:(j % 2 + 1) * 128]
                        WCK = ("wc", wi)
                        for g in range(NG):
                            gs = slice(g * 512, (g + 1) * 512)
                            it = j * NG + g
                            ii = it % 2
                            bsel = [(4 * it + r) % 6 for r in range(4)]
                            gkeys = [("nT", 4 * g + i) for i in range(4)]
                            for c in range(4):
                                mm(BANK[bsel[0]], wc[:, 16 + c, :], oaT[:, c, gs], c == 0, c == 3, OAK + [WCK], [BK(bsel[0])])
                            for c in range(4):
                                mm(BANK[bsel[1]], wc[:, 20 + c, :], obT[:, c, gs], c == 0, c == 3, OBK + [WCK], [BK(bsel[1])])
                            for c in range(8):
                                mm(BANK[bsel[2]], wc[:, c, :], nT[:, c, gs], c == 0, c == 7, gkeys + [WCK], [BK(bsel[2])])
                            for c in range(8):
                                mm(BANK[bsel[3]], wc[:, 8 + c, :], nT[:, c, gs], c == 0, c == 7, gkeys + [WCK], [BK(bsel[3])])
                            sg = sg_b[ii]
                            ACT(sg[:, 0, :], BANK[bsel[2]], AF.Sigmoid, [BK(bsel[2])], [("sg", ii, 0)])
                            ACT(sg[:, 1, :], BANK[bsel[3]], AF.Sigmoid, [BK(bsel[3])], [("sg", ii, 1)])
                            TT("dve", sg[:, 0, :], BANK[bsel[0]], sg[:, 0, :], ALU.mult, [BK(bsel[0]), ("sg", ii, 0)], [("sg", ii, 0)])
                            TT("dve", sg[:, 1, :], BANK[bsel[1]], sg[:, 1, :], ALU.mult, [BK(bsel[1]), ("sg", ii, 1)], [("sg", ii, 1)])
                            TT("pool", mTv[:, 4 * g:4 * g + 4, j, :], sg[:, 0, :].rearrange("p (t n) -> p t n", n=128),
                               sg[:, 1, :].rearrange("p (t n) -> p t n", n=128), ALU.add,
                               [("sg", ii, 0), ("sg", ii, 1)], [("mT", 4 * g + i) for i in range(4)])
                    if first:
                        dbg_dump("mT", mTv, [("mT", t) for t in range(NT)], [128, NT, 8, 128], BF16)
                P.barrier(scr[:, 0:1])
                if STOP == "C1":
                    return
                with ExitStack() as ph:
                    wo = sb("wo", [128, 8, D], BF16, ph)
                    rl_all = sb("rl_all", [128, NT, 36], F32, ph)
                    rs = sb("rs", [128, 8, NT], F32, ph)
                    r4a = sb("r4a", [128, 2, NT, 4], F32, ph)
                    r8 = sb("r8", [128, 6, NT, 8], F32, ph)
                    norm_bufs(ph, "c")
                    DMA("pool", wo[:], cw(w_out_d), "wo", [], ["wo"])
                    DMA("sp", gB[:], ffn_norm_d.partition_broadcast(128), "gB", [], ["gB"])
                    def HM(t):
                        i = t % 2
                        xt = NB["xt"][i]
                        DMA("sp", xt[:], x_d[s, t * 128:(t + 1) * 128, :], "xt%d" % i, [], [("xt", i)])
                        hps = PS2[i]
                        H2 = [BK(2 * i), BK(2 * i + 1)]
                        for half in range(2):
                            for c in range(8):
                                mm(hps[:, half * 512:(half + 1) * 512], mTv[:, t, c, :], wo[:, c, half * 512:(half + 1) * 512], c == 0, c == 7,
                                   [("mT", t), "wo"], [BK(2 * i + half)])
                        alias = []
                        if t >= NT // 2:
                            alias = [("mT", 2 * (t - NT // 2)), ("mT", 2 * (t - NT // 2) + 1)]
                        TT("dve", hres[:, t, :], hps[:, :], xt[:], ALU.add, H2 + [("xt", i)], [("h", t)] + alias)

                    HM(0)
                    for t in range(NT):
                        i = t % 2
                        tsl = slice(t * 128, (t + 1) * 128)
                        ix = norm_A(hres[:, t, :], [("h", t)], gB[:], "gB")
                        if t + 1 < NT:
                            HM(t + 1)
                        norm_B(ix, nT[:, :, tsl], ("nT", t))
                        rps = BANK[4 + i][:, 0:36]
                        for c in range(8):
                            mm(rps, nT[:, c, tsl], wr_bf[:, c, :], c == 0, False, [("nT", t), "wr_bf"], [BK(4 + i)])
                        mm(rps, ones_bf[:], rb_pad[:], False, True, ["ones_bf", "rb_pad"], [BK(4 + i)])
                        ACOPY(rl_all[:, t, :], rps, [BK(4 + i)], [("rl", t)])
                    RL = [("rl", t) for t in range(NT)]
                    RW = ["rw"]
                    gl = rl_all[:, :, 0:4]
                    el4 = rl_all[:, :, 4:36].rearrange("p t (g e) -> p t g e", e=8)

                    def bl(ap2, n):
                        return ap2.unsqueeze(2).to_broadcast([128, NT, n])

                    gmax, gsum, gp, emax, m2, ed_, w1, w2 = [rs[:, i_, :] for i_ in range(8)]
                    selg, gexp = r4a[:, 0, :, :], r4a[:, 1, :, :]
                    el, oh1, el2, oh2, cwt, tmp8 = [r8[:, i_, :, :] for i_ in range(6)]
                    RMAX(gmax, gl, RL, RW)
                    TT("dve", selg, gl, bl(gmax, 4), ALU.is_ge, RL + RW, RW)
                    TT("dve", gexp, gl, bl(gmax, 4), ALU.subtract, RL + RW, RW)
                    ACT(gexp, gexp, AF.Exp, RW, RW)
                    RSUM(gsum, gexp, RW, RW)
                    RECIP(gp, gsum, RW, RW)
                    TT("dve", el, el4[:, :, 0, :], bl(selg[:, :, 0], 8), ALU.mult, RL + RW, RW)
                    for g_ in range(1, 4):
                        TT("dve", tmp8, el4[:, :, g_, :], bl(selg[:, :, g_], 8), ALU.mult, RL + RW, RW)
                        TT("dve", el, el, tmp8, ALU.add, RW, RW)
                    RMAX(emax, el, RW, RW)
                    TT("dve", oh1, el, bl(emax, 8), ALU.is_ge, RW, RW)
                    STT("dve", el2, oh1, -1e30, el, ALU.mult, ALU.add, RW, RW)
                    RMAX(m2, el2, RW, RW)
                    TT("dve", oh2, el2, bl(m2, 8), ALU.is_ge, RW, RW)
                    TT("dve", ed_, emax, m2, ALU.subtract, RW, RW)
                    ACT(ed_, ed_, AF.Exp, RW, RW)
                    TS("dve", ed_, ed_, 1.0, None, ALU.add, None, RW, RW)
                    RECIP(w2, ed_, RW, RW)
                    TS("dve", w1, w2, -1.0, 1.0, ALU.mult, ALU.add, RW, RW)
                    TT("dve", w1, w1, gp, ALU.mult, RW, RW)
                    TT("dve", w2, w2, gp, ALU.mult, RW, RW)
                    TT("dve", cwt, oh1, bl(w1, 8), ALU.mult, RW, RW)
                    TT("dve", tmp8, oh2, bl(w2, 8), ALU.mult, RW, RW)
                    TT("dve", cwt, cwt, tmp8, ALU.add, RW, RW)
                    comb4 = comb[:, :, :].rearrange("p t (g e) -> p t g e", e=8)
                    for g_ in range(4):
                        TT("dve", comb4[:, :, g_, :], cwt, bl(selg[:, :, g_], 8), ALU.mult, RW, [("comb", t) for t in range(NT)])
                    if first:
                        dbg_dump("h1", hres, HK, [128, NT, D])
                        dbg_dump("comb", comb[:], [("comb", t) for t in range(NT)], [128, NT, 32])
                        dbg_dump("n2T", nT[:], NTK, [128, 8, T], BF16)
                P.barrier(scr[:, 0:1])

                if STOP == "C2":
                    return
                with ExitStack() as ph:
                    wgu_b = [sb("wgu%d" % i, [128, 8, 512], BF16, ph) for i in range(3)]
                    wdn_b = [sb("wdn%d" % i, [128, 2, D], BF16, ph) for i in range(3)]
                    sgl_b = [sb("sgl%d" % i, [128, 256], F32, ph) for i in range(2)]
                    he_b = [sb("he%d" % i, [128, 256], BF16, ph) for i in range(2)]
                    heT_b = [sb("heT%d" % i, [128, 2, 128], BF16, ph) for i in range(2)]
                    units = [(ex, t) for ex in range(NEX) for t in range(NT)]
                    NU = len(units)

                    def load_w(ex):
                        wi = ex % 3
                        DMA("pool", wgu_b[wi][:, :, 0:256], cw(w_gate_d[ex]), "wgu%d" % wi, [], [("wgu", wi)])
                        DMA("pool", wgu_b[wi][:, :, 256:512], cw(w_up_d[ex]), "wgu%d" % wi, [], [("wgu", wi)])
                        DMA("pool", wdn_b[wi][:], cw(w_down_d[ex]), "wdn%d" % wi, [], [("wdn", wi)])

                    def S1(u):
                        ex, t = units[u]
                        i, wi = u % 2, ex % 3
                        if t == 2 and ex + 2 < NEX:
                            load_w(ex + 2)
                        gups = BANK[4 + i]
                        for c in range(8):
                            mm(gups, nT[:, c, t * 128:(t + 1) * 128], wgu_b[wi][:, c, :], c == 0, c == 7, [("nT", t), ("wgu", wi)], [BK(4 + i)])
                        ACT(sgl_b[i][:], gups[:, 0:256], AF.Silu, [BK(4 + i)], [("sgl", i)])
                        STT("dve", he_b[i][:], gups[:, 256:512], comb[:, t, ex:ex + 1], sgl_b[i][:], ALU.mult, ALU.mult,
                            [BK(4 + i), ("comb", t), ("sgl", i)], [("he", i)])

                    def S2(u):
                        i = u % 2
                        pt = PT[i]
                        for c in range(2):
                            TR(pt[:, c * 128:(c + 1) * 128], he_b[i][:, c * 128:(c + 1) * 128], [("he", i)], [PTK(i)])
                        ACOPY(heT_b[i][:], pt[:, 0:256].rearrange("p (c n) -> p c n", n=128), [PTK(i)], [("heT", i)])

                    def S3(u):
                        ex, t = units[u]
                        i, wi = u % 2, ex % 3
                        yps = PS2[i]
                        for half in range(2):
                            for c in range(2):
                                mm(yps[:, half * 512:(half + 1) * 512], heT_b[i][:, c, :], wdn_b[wi][:, c, half * 512:(half + 1) * 512], c == 0, c == 1,
                                   [("heT", i), ("wdn", wi)], [BK(2 * i + half)])
                        TT("dve", hres[:, t, :], yps[:, :], hres[:, t, :], ALU.add, [BK(2 * i), BK(2 * i + 1), ("h", t)], [("h", t)])

                    for ex in range(min(2, NEX)):
                        load_w(ex)
                    for k in range(NU + 2):
                        if k < NU:
                            S1(k)
                        if 0 <= k - 1 < NU:
                            S2(k - 1)
                        if 0 <= k - 2 < NU:
                            S3(k - 2)
                    if first:
                        dbg_dump("h2", hres, HK, [128, NT, D])
                P.barrier(scr[:, 0:1])

                if STOP == "D":
                    return
                with ExitStack() as ph:
                    wpg = sb("wpg", [128, 8, D], BF16, ph)
                    wple = sb("wple", [128, 2, D], BF16, ph)
                    pt_b = [sb("ptile%d" % i, [128, 256], F32, ph) for i in range(3)]
                    pb_b = [sb("pb%d" % i, [128, 256], BF16, ph) for i in range(2)]
                    pT_b = [sb("pT%d" % i, [128, 2, 128], BF16, ph) for i in range(2)]
                    hb_b = [sb("hb%d" % i, [128, D], BF16, ph) for i in range(2)]
                    hT_b = [sb("hT%d" % i, [128, 8, 128], BF16, ph) for i in range(2)]
                    en_b = [sb("en%d" % i, [128, D], F32, ph) for i in range(1)]
                    sgt_b = [sb("sgt%d" % i, [128, D], F32, ph) for i in range(2)]
                    ot_b = [sb("ot%d" % i, [128, D], F32, ph) for i in range(1)]
                    junk_e = sb("junk_e", [128, D], BF16, ph)
                    est_b = [sb("est%d" % i, [128, 8], F32, ph) for i in range(2)]
                    DMA("pool", wpg[:], cw(w_pg_d), "wpg", [], ["wpg"])
                    DMA("pool", wple[:], cw(w_ple_d), "wple", [], ["wple"])
                    DMA("sp", gA[:], ple_norm_d.partition_broadcast(128), "gA", [], ["gA"])
                    DMA("sp", gB[:], final_norm_d.partition_broadcast(128), "gB", [], ["gB"])
                    def E1a(t):
                        i = t % 2
                        VCOPY("dve", pb_b[i][:], pt_b[t % 3][:], [("ptile", t % 3)], [("pb", i)])
                        VCOPY("dve", hb_b[i][:], hres[:, t, :], [("h", t)], [("hb", i)])

                    def E1b(t):
                        i = t % 2
                        transpose_to(pb_b[i], ("pb", i), 2, pT_b[i][:, :, :], ("pT", i))
                        transpose_to(hb_b[i], ("hb", i), 8, hT_b[i][:, :, :], ("hT", i))

                    def E2(t):
                        i = t % 2
                        pT, hT, en, sgt, est = pT_b[i], hT_b[i], en_b[0], sgt_b[i], est_b[i]
                        EK, SG = ("est", i), ("sgt", i)
                        eps_ = PS2[0]
                        for half in range(2):
                            for c in range(2):
                                mm(eps_[:, half * 512:(half + 1) * 512], pT[:, c, :], wple[:, c, half * 512:(half + 1) * 512], c == 0, c == 1, [("pT", i), "wple"], [BK(half)])
                        gps = PS2[1]
                        for half in range(2):
                            for c in range(8):
                                mm(gps[:, half * 512:(half + 1) * 512], hT[:, c, :], wpg[:, c, half * 512:(half + 1) * 512], c == 0, c == 7, [("hT", i), "wpg"], [BK(2 + half)])
                        MSET(est[:, 0:4], 0.0, [EK])
                        ACT(junk_e[:], eps_[:, :], AF.Square, [BK(0), BK(1), EK], ["junk_e", EK], accum_out=est[:, 0:1])
                        rstd_ops(est[:, 1:2], est[:, 0:1], 1.0 / D, [EK])
                        STT("dve", en[:], eps_[:, :], est[:, 1:2], gA[:], ALU.mult, ALU.mult, [BK(0), BK(1), EK, "gA"], [("en", 0)])
                        ACT(sgt[:], gps[:, :], AF.Sigmoid, [BK(2), BK(3)], [SG])
                        TT("dve", sgt[:], sgt[:], en[:], ALU.mult, [SG, ("en", 0)], [SG])
                        TT("pool", sgt[:], sgt[:], hres[:, t, :], ALU.add, [SG, ("h", t)], [SG])

                    def E3(t):
                        i = t % 2
                        sgt, ot, est = sgt_b[i], ot_b[0], est_b[i]
                        EK, SG = ("est", i), ("sgt", i)
                        ACT(junk_e[:], sgt[:], AF.Square, [SG, EK], ["junk_e", EK], accum_out=est[:, 2:3])
                        rstd_ops(est[:, 3:4], est[:, 2:3], 1.0 / D, [EK])
                        STT("dve", ot[:], sgt[:], est[:, 3:4], gB[:], ALU.mult, ALU.mult, [SG, EK, "gB"], [("ot", 0)])
                        DMA("sp", out_d[s, t * 128:(t + 1) * 128, :], ot[:], "out0", [("ot", 0)], [("out", 0)])

                    def E0(t):
                        DMA("pool", pt_b[t % 3][:], p_d[s, t * 128:(t + 1) * 128, :], "ptile%d" % (t % 3), [], [("ptile", t % 3)])

                    E0(0)
                    if NT > 1:
                        E0(1)
                    E1a(0)
                    E1b(0)
                    for k in range(NT + 1):
                        if k + 2 < NT:
                            E0(k + 2)
                        if k >= 1:
                            E3(k - 1)
                        if k + 1 < NT:
                            E1a(k + 1)
                        if k < NT:
                            E2(k)
                        if k + 1 < NT:
                            E1b(k + 1)
            P.barrier(scr[:, 0:1])

        for s in range(NSEQ):
            seq_body(s)

        fk = [("out", 0)] + [("dbgout", n) for n in dbg_outs]
        counts = P.emit(fk)
    build.info = {"nops": len(P.ops), "signals": counts, "dbg": list(dbg_outs)}
    return nc


_CACHE = {}


def _weights_map(inp):
    g = lambda k: np.ascontiguousarray(np.asarray(inp[k]))
    return {
        "attn_norm": g("attn_norm")[0], "w_in": g("w_in")[0], "w_a2": g("w_a2")[0], "b_a": g("b_a")[0],
        "gla_norm": g("gla_norm")[0], "lq1": g("lambda_q1")[0], "lk1": g("lambda_k1")[0],
        "lq2": g("lambda_q2")[0], "lk2": g("lambda_k2")[0], "diff_subln": g("diff_subln")[0],
        "w_ba": g("w_branch_a")[0], "w_bb": g("w_branch_b")[0], "w_out": g("w_out")[0],
        "ffn_norm": g("ffn_norm")[0], "w_rg": g("w_router_group")[0], "b_rg": g("b_router_group")[0],
        "w_re": g("w_router_expert")[0], "b_re": g("b_router_expert")[0],
        "w_gate": g("w_gate")[0], "w_up": g("w_up")[0], "w_down": g("w_down")[0],
        "w_ple": g("w_ple")[0], "ple_norm": g("ple_norm")[0], "w_pg": g("w_ple_gate")[0],
        "final_norm": g("final_norm"), "cst": make_consts(),
    }


def kernel(**inputs):
    x = np.asarray(inputs["x"], dtype=np.float32)
    p = np.asarray(inputs["p"], dtype=np.float32)[0]
    pos = np.asarray(inputs["positions"], dtype=np.int32)
    B, T, _ = x.shape
    ncores = 8
    nseq = B // ncores
    key = (T, nseq)
    if key not in _CACHE:
        _CACHE[key] = build(T=T, NSEQ=nseq)
    nc = _CACHE[key]
    w = _weights_map(inputs)
    in_maps = []
    for c in range(ncores):
        m = dict(w)
        m["x"] = np.ascontiguousarray(x[c * nseq:(c + 1) * nseq])
        m["p"] = np.ascontiguousarray(p[c * nseq:(c + 1) * nseq])
        m["pos"] = np.ascontiguousarray(pos[c * nseq:(c + 1) * nseq])
        in_maps.append(m)
    res = run_bass_kernel_spmd(nc, in_maps, core_ids=list(range(ncores)))
    out = np.concatenate([np.asarray(r["out"]) for r in res.results], axis=0)
    return out.astype(np.float32, copy=False)
```
